# Optimizing a Trainium2 kernel written in Bass

```python
import math
import jax, jax.numpy as jnp
from jax import lax
import numpy as np

D_MODEL = 1024
BATCH = 8
SEQ = 4096
DEPTH = 1

HEAD_DIM = 64
N_HEADS_A = 8
N_IDX_HEADS = 8
IDX_DIM = 32
TOPK_TOKENS = 256
N_HEADS_B = 8
N_KV_GROUPS_B = 2
HEADS_PER_GROUP = N_HEADS_B // N_KV_GROUPS_B
CMP_BLOCK = 32
CMP_STRIDE = 16
CMP_HIDDEN = 128
SLC_BLOCK = 64
N_SLC_BLOCKS = 16
WINDOW = 512
FORCED_SCORE = 1.0e4
N_BUCKETS = 32
MAX_DISTANCE = 128
D_FF = 4 * D_MODEL
Q_BLOCK_A = 128
Q_BLOCK_B = 64
EPS = 1e-6

W_QA = N_HEADS_A * HEAD_DIM
W_KA = HEAD_DIM
W_VA = HEAD_DIM
W_QI = N_IDX_HEADS * IDX_DIM
W_KI = IDX_DIM
W_WI = N_IDX_HEADS
W_QB = N_HEADS_B * HEAD_DIM
W_KVB = 6 * N_KV_GROUPS_B * HEAD_DIM
W_GB = 3 * N_HEADS_B
W_GATE = 2 * D_MODEL
D_IN = W_QA + W_KA + W_VA + W_QI + W_KI + W_WI + W_QB + W_KVB + W_GB + W_GATE

kernel_name = "hybrid_dsa_nsa_gated_block"


def rmsnorm(x, g):
    xf = x.astype(jnp.float32)
    y = xf * lax.rsqrt(jnp.mean(xf * xf, axis=-1, keepdims=True) + EPS)
    return (y * g.astype(jnp.float32)).astype(x.dtype)


def t5_bucket(dist):
    n = jnp.maximum(dist, 0)
    max_exact = N_BUCKETS // 2
    nf = jnp.maximum(n, 1).astype(jnp.float32)
    large = max_exact + (jnp.log(nf / max_exact) / math.log(MAX_DISTANCE / max_exact)
                         * (N_BUCKETS - max_exact)).astype(jnp.int32)
    large = jnp.minimum(large, N_BUCKETS - 1)
    return jnp.where(n < max_exact, n, large)


def masked_softmax(logits, mask):
    lf = jnp.where(mask, logits.astype(jnp.float32), -jnp.inf)
    m = jnp.max(lf, axis=-1, keepdims=True)
    m = jnp.where(jnp.isfinite(m), m, 0.0)
    e = jnp.where(mask, jnp.exp(lf - m), 0.0)
    return e / jnp.maximum(jnp.sum(e, axis=-1, keepdims=True), 1e-30)


def dsa_mixer(q_a, k_a, v_a, q_i, k_i, w_i, table_a):
    B, S = q_a.shape[:2]
    k_top = min(TOPK_TOKENS, S // 4)
    n_blk = S // Q_BLOCK_A
    scale = HEAD_DIM ** -0.5
    w_scale = (N_IDX_HEADS ** -0.5) * (IDX_DIM ** -0.5)
    key_pos = jnp.arange(S)
    gather = jax.vmap(lambda kb, ib: kb[ib])

    def block(i):
        q0 = i * Q_BLOCK_A
        t = q0 + jnp.arange(Q_BLOCK_A)
        qi = lax.dynamic_slice_in_dim(q_i, q0, Q_BLOCK_A, axis=1)
        wi = lax.dynamic_slice_in_dim(w_i, q0, Q_BLOCK_A, axis=1).astype(jnp.float32) * w_scale
        s_h = jnp.einsum('bqhd,bsd->bqhs', qi, k_i).astype(jnp.float32)
        score = jnp.einsum('bqh,bqhs->bqs', wi, jax.nn.relu(s_h))
        causal = key_pos[None, :] <= t[:, None]
        score = jnp.where(causal[None], score, -jnp.inf)
        _, idx = lax.top_k(score, k_top)
        k_sel = gather(k_a, idx)
        v_sel = gather(v_a, idx)
        qa = lax.dynamic_slice_in_dim(q_a, q0, Q_BLOCK_A, axis=1)
        dist = t[None, :, None] - idx
        bias = table_a[t5_bucket(dist)].transpose(0, 1, 3, 2)
        logits = jnp.einsum('bqhd,bqkd->bqhk', qa, k_sel) * scale + bias
        p = masked_softmax(logits, (dist >= 0)[:, :, None, :])
        o = jnp.einsum('bqhk,bqkd->bqhd', p.astype(v_sel.dtype), v_sel)
        return o.reshape(B, Q_BLOCK_A, N_HEADS_A * HEAD_DIM)

    out = lax.map(block, jnp.arange(n_blk))
    return out.transpose(1, 0, 2, 3).reshape(B, S, N_HEADS_A * HEAD_DIM)


def compress_blocks(kv, pe, w1, w2):
    B, S, G, D = kv.shape
    n_c = (S - CMP_BLOCK) // CMP_STRIDE + 1
    idx = jnp.arange(n_c)[:, None] * CMP_STRIDE + jnp.arange(CMP_BLOCK)[None, :]
    blk = kv[:, idx] + pe[None, None, :, None, :]
    blk = blk.transpose(0, 1, 3, 2, 4).reshape(B, n_c, G, CMP_BLOCK * D)
    return jax.nn.gelu(blk @ w1) @ w2


def nsa_mixer(q, kc, vc, ks, vs, kw, vw, gates, pe_k, w1_k, w2_k, pe_v, w1_v, w2_v, table_b):
    B, S = q.shape[:2]
    G, R, D = N_KV_GROUPS_B, HEADS_PER_GROUP, HEAD_DIM
    QB = Q_BLOCK_B
    scale = D ** -0.5
    q = q.reshape(B, S, G, R, D)
    gates = jax.nn.sigmoid(gates.astype(jnp.float32)).reshape(B, S, 3, G, R)
    kc_cmp = compress_blocks(kc, pe_k, w1_k, w2_k)
    vc_cmp = compress_blocks(vc, pe_v, w1_v, w2_v)
    n_c = kc_cmp.shape[1]
    n_s = S // SLC_BLOCK
    n_sel = min(N_SLC_BLOCKS, n_s)
    cmp_start = jnp.arange(n_c) * CMP_STRIDE
    cmp_end = cmp_start + CMP_BLOCK - 1
    slc_start = jnp.arange(n_s) * SLC_BLOCK
    overlap = jnp.clip(jnp.minimum(cmp_start[:, None] + CMP_BLOCK, slc_start[None, :] + SLC_BLOCK)
                       - jnp.maximum(cmp_start[:, None], slc_start[None, :]), 0, None
                       ).astype(jnp.float32) / CMP_BLOCK
    ks_blk = ks.reshape(B, n_s, SLC_BLOCK, G, D).transpose(0, 3, 1, 2, 4)
    vs_blk = vs.reshape(B, n_s, SLC_BLOCK, G, D).transpose(0, 3, 1, 2, 4)
    gather = jax.vmap(jax.vmap(lambda kb, ib: kb[ib]))
    pad = ((0, 0), (WINDOW, 0), (0, 0), (0, 0))
    kw_p = jnp.pad(kw, pad)
    vw_p = jnp.pad(vw, pad)
    tg = table_b.reshape(N_BUCKETS, G, R).transpose(1, 0, 2)
    g_idx = jnp.arange(G)[None, :, None, None]
    blk_id = jnp.arange(n_s)
    KW = WINDOW + QB

    def block(i):
        q0 = i * QB
        t = q0 + jnp.arange(QB)
        qb = lax.dynamic_slice_in_dim(q, q0, QB, axis=1)
        gb = lax.dynamic_slice_in_dim(gates, q0, QB, axis=1)
        dist_c = t[:, None] - cmp_end[None, :]
        bias_c = table_b[t5_bucket(dist_c)].reshape(QB, n_c, G, R).transpose(0, 2, 3, 1)
        logit_c = jnp.einsum('bqgrd,bcgd->bqgrc', qb, kc_cmp) * scale + bias_c
        p_c = masked_softmax(logit_c, (dist_c >= 0)[:, None, None, :])
        o_c = jnp.einsum('bqgrc,bcgd->bqgrd', p_c.astype(vc_cmp.dtype), vc_cmp)
        imp = jnp.einsum('bqgrc,cn->bqgn', p_c, overlap)
        cur = t // SLC_BLOCK
        valid = blk_id[None, :] <= cur[:, None]
        forced = ((blk_id[None, :] == 0) | (blk_id[None, :] == cur[:, None])
                  | (blk_id[None, :] == cur[:, None] - 1))
        score = jnp.where(valid[None, :, None, :],
                          jnp.where(forced[None, :, None, :], FORCED_SCORE, imp), -jnp.inf)
        _, sel = lax.top_k(score, n_sel)
        sel_t = sel.transpose(0, 2, 1, 3)
        k_sel = gather(ks_blk, sel_t).reshape(B, G, QB, n_sel * SLC_BLOCK, D)
        v_sel = gather(vs_blk, sel_t).reshape(B, G, QB, n_sel * SLC_BLOCK, D)
        pos_s = (sel_t[..., None] * SLC_BLOCK + jnp.arange(SLC_BLOCK)).reshape(B, G, QB, n_sel * SLC_BLOCK)
        dist_s = t[None, None, :, None] - pos_s
        bias_s = tg[g_idx, t5_bucket(dist_s)].transpose(0, 1, 2, 4, 3)
        logit_s = jnp.einsum('bqgrd,bgqkd->bgqrk', qb, k_sel) * scale + bias_s
        p_s = masked_softmax(logit_s, (dist_s >= 0)[:, :, :, None, :])
        o_s = jnp.einsum('bgqrk,bgqkd->bqgrd', p_s.astype(v_sel.dtype), v_sel)
        kwb = lax.dynamic_slice_in_dim(kw_p, q0, KW, axis=1)
        vwb = lax.dynamic_slice_in_dim(vw_p, q0, KW, axis=1)
        pos_w = q0 - WINDOW + jnp.arange(KW)
        dist_w = t[:, None] - pos_w[None, :]
        mask_w = (dist_w >= 0) & (dist_w < WINDOW) & (pos_w[None, :] >= 0)
        bias_w = table_b[t5_bucket(dist_w)].reshape(QB, KW, G, R).transpose(0, 2, 3, 1)
        logit_w = jnp.einsum('bqgrd,bkgd->bqgrk', qb, kwb) * scale + bias_w
        p_w = masked_softmax(logit_w, mask_w[:, None, None, :])
        o_w = jnp.einsum('bqgrk,bkgd->bqgrd', p_w.astype(vwb.dtype), vwb)
        o = (gb[:, :, 0, :, :, None] * o_c + gb[:, :, 1, :, :, None] * o_s
             + gb[:, :, 2, :, :, None] * o_w)
        return o.astype(q.dtype).reshape(B, QB, G * R * D)

    out = lax.map(block, jnp.arange(S // QB))
    return out.transpose(1, 0, 2, 3).reshape(B, S, N_HEADS_B * HEAD_DIM)


def setup_inputs(seed: int = 0) -> dict:
    key = jax.random.key(seed)
    ks = jax.random.split(key, 20)
    f32 = jnp.float32
    nrm = lambda k, shape, s: jax.random.normal(k, shape, f32) * s
    L, D = CMP_BLOCK, HEAD_DIM
    return {
        "x": nrm(ks[0], (BATCH, SEQ, D_MODEL), 1.0),
        "norm_mix": 1.0 + nrm(ks[1], (DEPTH, D_MODEL), 0.02),
        "w_in": nrm(ks[2], (DEPTH, D_MODEL, D_IN), D_MODEL ** -0.5),
        "cmp_pe_k": nrm(ks[3], (DEPTH, L, D), 0.1),
        "cmp_w1_k": nrm(ks[4], (DEPTH, L * D, CMP_HIDDEN), (L * D) ** -0.5),
        "cmp_w2_k": nrm(ks[5], (DEPTH, CMP_HIDDEN, D), CMP_HIDDEN ** -0.5),
        "cmp_pe_v": nrm(ks[6], (DEPTH, L, D), 0.1),
        "cmp_w1_v": nrm(ks[7], (DEPTH, L * D, CMP_HIDDEN), (L * D) ** -0.5),
        "cmp_w2_v": nrm(ks[8], (DEPTH, CMP_HIDDEN, D), CMP_HIDDEN ** -0.5),
        "rel_bias": nrm(ks[9], (N_BUCKETS, N_HEADS_A + N_HEADS_B), 0.5),
        "w_branch_a": nrm(ks[10], (DEPTH, W_QA, D_MODEL), W_QA ** -0.5),
        "w_branch_b": nrm(ks[11], (DEPTH, W_QB, D_MODEL), W_QB ** -0.5),
        "w_out": nrm(ks[12], (DEPTH, D_MODEL, D_MODEL), D_MODEL ** -0.5),
        "norm_mlp": 1.0 + nrm(ks[13], (DEPTH, D_MODEL), 0.02),
        "w_mlp_in": nrm(ks[14], (DEPTH, D_MODEL, D_FF), D_MODEL ** -0.5),
        "w_mlp_out": nrm(ks[15], (DEPTH, D_FF, D_MODEL), D_FF ** -0.5),
        "norm_final": 1.0 + nrm(ks[16], (D_MODEL,), 0.02),
    }


def reference(x, norm_mix, w_in, cmp_pe_k, cmp_w1_k, cmp_w2_k, cmp_pe_v, cmp_w1_v, cmp_w2_v,
              rel_bias, w_branch_a, w_branch_b, w_out, norm_mlp, w_mlp_in, w_mlp_out, norm_final):
    B, S, _ = x.shape
    G, D = N_KV_GROUPS_B, HEAD_DIM
    widths = [W_QA, W_KA, W_VA, W_QI, W_KI, W_WI, W_QB, W_KVB, W_GB, W_GATE]
    split_pts = [int(v) for v in np.cumsum(widths)[:-1]]
    table_a = rel_bias[:, :N_HEADS_A]
    table_b = rel_bias[:, N_HEADS_A:]
    for l in range(DEPTH):
        h = rmsnorm(x, norm_mix[l])
        proj = h @ w_in[l]
        q_a, k_a, v_a, q_i, k_i, w_i, q_b, kv_b, g_b, g_br = jnp.split(proj, split_pts, axis=-1)
        o_a = dsa_mixer(q_a.reshape(B, S, N_HEADS_A, D), k_a, v_a,
                        q_i.reshape(B, S, N_IDX_HEADS, IDX_DIM), k_i, w_i, table_a)
        kv_b = kv_b.reshape(B, S, 6, G, D)
        o_b = nsa_mixer(q_b.reshape(B, S, N_HEADS_B, D), kv_b[:, :, 0], kv_b[:, :, 1],
                        kv_b[:, :, 2], kv_b[:, :, 3], kv_b[:, :, 4], kv_b[:, :, 5], g_b,
                        cmp_pe_k[l], cmp_w1_k[l], cmp_w2_k[l], cmp_pe_v[l], cmp_w1_v[l],
                        cmp_w2_v[l], table_b)
        gate_a, gate_b = jnp.split(g_br, 2, axis=-1)
        mix = (jax.nn.sigmoid(gate_a) * (o_a @ w_branch_a[l])
               + jax.nn.sigmoid(gate_b) * (o_b @ w_branch_b[l]))
        x = x + mix @ w_out[l]
        h = rmsnorm(x, norm_mlp[l])
        x = x + jnp.square(jax.nn.relu(h @ w_mlp_in[l])) @ w_mlp_out[l]
    return rmsnorm(x, norm_final)
```

```python
import math
from contextlib import ExitStack

import numpy as np
import concourse.bass as bass
import concourse.mybir as mybir
from concourse.bass_utils import run_bass_kernel_spmd

F32 = mybir.dt.float32
BF16 = mybir.dt.bfloat16
U8 = mybir.dt.uint8
ALU = mybir.AluOpType
AF = mybir.ActivationFunctionType
AX = mybir.AxisListType

S = 4096
DM = 1024
NT = S // 128
DFF = 4096
EPS = 1e-6
NEG = -30000.0
EPOCH = 6000
NDMASEM = 24
INORDER = ("pe", "act", "dve", "pool")


class Ev:
    __slots__ = ("eng", "idx", "sem", "val")

    def __init__(self, eng, idx=None, sem=None, val=None):
        self.eng, self.idx, self.sem, self.val = eng, idx, sem, val


class Buf:
    __slots__ = ("name", "w", "r", "rd")

    def __init__(self, name):
        self.name, self.w, self.r, self.rd = name, None, {}, []


class KB:
    def __init__(self, nc):
        self.nc = nc
        self.ops = {e: [] for e in INORDER + ("sp",)}
        self.nsig = {e: 0 for e in INORDER}
        self.dma_next = 0
        self.dma_val = [0] * NDMASEM
        self.dma_out = []
        self.all_dma = []

    def _deps(self, reads, writes):
        deps = []
        for b in reads:
            if b.w is not None:
                deps.append(b.w)
        for b in writes:
            if b.w is not None:
                deps.append(b.w)
            deps.extend(b.r.values())
            deps.extend(b.rd)
        return deps

    def _mark(self, ev, reads, writes):
        for b in reads:
            if ev.eng == "dma":
                b.rd.append(ev)
            else:
                b.r[ev.eng] = ev
        for b in writes:
            b.w = ev
            b.r = {}
            b.rd = []

    def op(self, eng, emit, reads=(), writes=(), signal=True, extra=()):
        deps = self._deps(reads, writes) + list(extra)
        deps = [d for d in deps if not (d.eng == eng and d.idx >= self.nsig[eng])]
        ev = Ev(eng, self.nsig[eng])
        if signal:
            self.nsig[eng] += 1
        self.ops[eng].append(("op", emit, deps, ev if signal else None))
        self._mark(ev, reads, writes)
        return ev

    def dma(self, out, in_, reads=(), writes=(), q="sp", extra=()):
        deps = self._deps(reads, writes) + list(extra)
        k = self.dma_next
        self.dma_next = (k + 1) % NDMASEM
        prev = self.dma_val[k]
        self.dma_val[k] = prev + 16
        ev = Ev("dma", None, k, prev + 16)
        self.ops[q].append(("dma", (out, in_), deps, ev, prev))
        self._mark(ev, reads, writes)
        self.dma_out.append(ev)
        self.all_dma.append(ev)
        return ev

    def barrier(self):
        evs = [Ev(e, self.nsig[e] - 1) for e in INORDER if self.nsig[e] > 0]
        evs += self.dma_out
        self.dma_out = []
        for e in INORDER + ("sp",):
            self.ops[e].append(("wait", None, list(evs), None))

    def check_deadlock(self):
        engs = list(self.ops.keys())
        ptr = {e: 0 for e in engs}
        done_sig = {e: 0 for e in INORDER}
        done_dma = set()
        progress = True
        while progress:
            progress = False
            for e in engs:
                while ptr[e] < len(self.ops[e]):
                    rec = self.ops[e][ptr[e]]
                    deps = rec[2]
                    ok = True
                    for d in deps:
                        if d.eng == "dma":
                            if id(d) not in done_dma:
                                ok = False
                                break
                        elif d.idx >= done_sig[d.eng]:
                            ok = False
                            break
                    if not ok:
                        break
                    if rec[0] == "op" and rec[3] is not None:
                        done_sig[e] += 1
                    elif rec[0] == "dma":
                        done_dma.add(id(rec[3]))
                    ptr[e] += 1
                    progress = True
        stuck = {e: (ptr[e], len(self.ops[e])) for e in engs if ptr[e] < len(self.ops[e])}
        return stuck, done_sig

    def replay(self, stack):
        nc = self.nc
        sems = {}
        for e in INORDER:
            n = self.nsig[e] // EPOCH + 1
            sems[e] = [stack.enter_context(nc.semaphore(f"s_{e}{i}")) for i in range(n)]
        dsem = [stack.enter_context(nc.semaphore(f"s_d{i}")) for i in range(NDMASEM)]
        final_evs = [Ev(e, self.nsig[e] - 1) for e in INORDER if self.nsig[e] > 0] + self.all_dma[-64:]
        self.ops["sp"].append(("wait", None, final_evs, None))
        block = stack.enter_context(nc.Block())

        def run(eng_name):
            def body(e):
                waited = {a: -1 for a in INORDER}
                wd = [0] * NDMASEM

                def do_waits(deps):
                    need = {}
                    for d in deps:
                        if d.eng == "dma":
                            if d.val > wd[d.sem]:
                                wd[d.sem] = d.val
                                e.wait_ge(dsem[d.sem], d.val)
                        else:
                            if d.idx > waited[d.eng]:
                                need[d.eng] = max(need.get(d.eng, -1), d.idx)
                    for a, idx in need.items():
                        waited[a] = idx
                        e.wait_ge(sems[a][idx // EPOCH], idx % EPOCH + 1)

                for rec in self.ops[eng_name]:
                    kind = rec[0]
                    if kind == "wait":
                        do_waits(rec[2])
                    elif kind == "op":
                        _, emit, deps, ev = rec
                        do_waits(deps)
                        ins = emit(e)
                        if ev is not None:
                            ins.then_inc(sems[ev.eng][ev.idx // EPOCH], 1)
                    else:
                        _, (out, in_), deps, ev, prev = rec
                        do_waits(deps)
                        if prev > wd[ev.sem]:
                            wd[ev.sem] = prev
                            e.wait_ge(dsem[ev.sem], prev)
                        e.dma_start(out=out, in_=in_).then_inc(dsem[ev.sem], 16)
            return body

        block.tensor(run("pe"))
        block.scalar(run("act"))
        block.vector(run("dve"))
        block.gpsimd(run("pool"))
        block.sync(run("sp"))


class Arena:
    def __init__(self, ap, nwords):
        self.ap, self.n, self.top = ap, nwords, 0

    def alloc(self, shape, dt, name=None):
        free = int(np.prod(shape[1:]))
        bpe = {F32: 4, BF16: 2, U8: 1}[dt]
        words = (free * bpe + 3) // 4
        off = self.top
        self.top += words
        assert self.top <= self.n, f"SBUF arena overflow {self.top} > {self.n} ({name})"
        v = self.ap[:, off:off + words]
        if dt != F32:
            v = v.bitcast(dt)
        v = v[:, 0:free]
        if len(shape) == 3:
            v = v.rearrange("p (a b) -> p a b", b=shape[2])
        elif len(shape) == 4:
            v = v.rearrange("p (a b c) -> p a b c", b=shape[2], c=shape[3])
        return v

    def mark(self):
        return self.top

    def release(self, m):
        self.top = m


def build_program(stage):
    nc = bass.Bass("TRN2", target_bir_lowering=False)
    D = {}

    def din(name, shape, dt=F32):
        D[name] = nc.dram_tensor(name, list(shape), dt, kind="ExternalInput").ap()
        return D[name]

    x = din("x", [S, DM])
    gmix = din("gmix", [128, 8])
    gmlp = din("gmlp", [128, 8])
    gfin = din("gfin", [1, DM])
    w1d = din("w_mlp_in", [DM, DFF])
    w2d = din("w_mlp_out", [DFF, DM])
    identd = din("ident", [128, 128])
    out = nc.dram_tensor("out", [S, DM], F32, kind="ExternalOutput").ap()
    x2d = nc.dram_tensor("x2_scratch", [S, DM], F32, kind="Internal").ap()

    stack = ExitStack()
    with stack:
        NW = 53000
        arena_t = stack.enter_context(nc.sbuf_tensor("arena", [128, NW], F32))
        A = Arena(arena_t, NW)
        banks = [stack.enter_context(nc.psum_tensor(f"bank{i}", [128, 512], F32)) for i in range(8)]
        bankB = [Buf(f"bank{i}") for i in range(8)]
        K = KB(nc)

        ident_f = A.alloc([128, 128], F32)
        ident = A.alloc([128, 128], BF16)
        gmix_s = A.alloc([128, 8], F32)
        gmlp_s = A.alloc([128, 8], F32)
        gfin_s = A.alloc([128, DM], F32)
        cB = Buf("consts")
        K.dma(ident_f, identd, writes=[cB])
        K.dma(gmix_s, gmix, writes=[cB])
        K.dma(gmlp_s, gmlp, writes=[cB])
        K.dma(gfin_s, gfin.partition_broadcast(128), writes=[cB])
        K.op("dve", lambda e: e.tensor_copy(out=ident, in_=ident_f), reads=[cB], writes=[cB])

        def rms_scale(ss, rs, n, bufs):
            K.op("dve", lambda e: e.tensor_scalar(out=rs, in0=ss, scalar1=1.0 / DM, scalar2=EPS,
                                                  op0=ALU.mult, op1=ALU.add), reads=bufs, writes=bufs)
            K.op("act", lambda e: e.activation(out=rs, in_=rs, func=AF.Sqrt), reads=bufs, writes=bufs)
            K.op("dve", lambda e: e.reciprocal(out=rs, in_=rs), reads=bufs, writes=bufs)

        def normT_tile(xt, xtB, gT, dst3, dstB, wk):
            sq, ss, rs, xn, sB = wk["sq"], wk["ss"], wk["rs"], wk["xn"], wk["B"]
            K.op("act", lambda e: e.activation(out=sq, in_=xt, func=AF.Square, accum_out=ss),
                 reads=[xtB], writes=[sB])
            rms_scale(ss, rs, 1, [sB])
            K.op("dve", lambda e: e.tensor_scalar(out=xn, in0=xt, scalar1=rs, scalar2=None, op0=ALU.mult),
                 reads=[xtB, sB], writes=[sB])
            bk = wk["bank"]
            pv = banks[bk][:, 0:512].bitcast(BF16)
            for c in range(8):
                K.op("pe", lambda e, c=c: e.transpose(pv[:, c * 128:(c + 1) * 128], xn[:, c * 128:(c + 1) * 128], ident),
                     reads=[sB, cB], writes=[bankB[bk]], signal=(c == 7))
            K.op("dve", lambda e: e.tensor_tensor(out=dst3, in0=pv.rearrange("p (a b) -> p a b", b=128),
                                                  in1=gT.unsqueeze(2).to_broadcast([128, 8, 128]), op=ALU.mult),
                 reads=[bankB[bk], cB], writes=[dstB])


        NIT = 17
        xv = x.rearrange("(n p) d -> p n d", p=128)
        x2v_w = x2d.rearrange("(n p) d -> p n d", p=128)
        if stage >= 2:
            wfa = din("wfa", [8, 128, 1024])
            wta = din("wta", [128, 8 * 72])
            dgT = din("dgT", [128, 16 * 2 * 128])
            c31d = din("c31", [1, 16])
            cnegTd = din("cnegT", [128, 128])
            ctokd = din("ctok", [128, 128])
            p2d = din("pow2", [1, NIT])
            wbad = din("wba", [512, DM])
            wbbd = din("wbb", [512, DM])
            wod = din("wo", [DM, DM])
            wgd = din("wg", [DM, 2048])
            if DEBUG:
                dbg_oa = nc.dram_tensor("dbg_oa", [S, 512], BF16, kind="ExternalOutput").ap()
                dbg_ob = nc.dram_tensor("dbg_ob", [S, 512], F32, kind="ExternalOutput").ap()
                dbg_x2 = nc.dram_tensor("dbg_x2", [S, DM], F32, kind="ExternalOutput").ap()
                dbg_mix = nc.dram_tensor("dbg_mix", [S, DM], F32, kind="ExternalOutput").ap()
                dbg_sig = nc.dram_tensor("dbg_sig", [S, 2048], F32, kind="ExternalOutput").ap()
                dbg_ya = nc.dram_tensor("dbg_ya", [S, DM], F32, kind="ExternalOutput").ap()
                dbg_hT = nc.dram_tensor("dbg_hT", [128, 1024], F32, kind="ExternalOutput").ap()
                dbg_z = nc.dram_tensor("dbg_z", [128, 512], F32, kind="ExternalOutput").ap()
                dbg_wg = nc.dram_tensor("dbg_wg", [128, 2048], F32, kind="ExternalOutput").ap()

            c31 = A.alloc([128, 16], F32)
            zeros_sb = A.alloc([128, 260], F32)
            K.op("pool", lambda e: e.memset(zeros_sb, 0.0), writes=[cB])
            ctok = A.alloc([128, 128], F32)
            pow2 = A.alloc([128, NIT], F32)
            Dp = A.alloc([128, 16, 2, 128], BF16)
            K.dma(c31, c31d.partition_broadcast(128), writes=[cB])
            K.dma(ctok, ctokd, writes=[cB])
            K.dma(pow2, p2d.partition_broadcast(128), writes=[cB])
            mP = A.mark()
            if stage == 4:
                oa_scr = din("t_oa", [S, 512], BF16).rearrange("(n p) d -> p n d", p=128)
                ob_scr = din("t_ob", [S, 512], BF16).rearrange("(n p) d -> p n d", p=128)
            else:
                oa_scr = nc.dram_tensor("oa_scr", [S, 512], BF16, kind="Internal").ap().rearrange("(n p) d -> p n d", p=128)
                ob_scr = nc.dram_tensor("ob_scr", [S, 512], BF16, kind="Internal").ap().rearrange("(n p) d -> p n d", p=128)
            mB0 = A.mark()
            dgs = A.alloc([128, 16, 2, 128], F32)
            cnegT = A.alloc([128, 128], F32)
            K.dma(dgs, dgT.rearrange("p (a b c) -> p a b c", b=2, c=128), writes=[cB])
            K.dma(cnegT, cnegTd, writes=[cB])
            for hh in range(16):
                K.op("dve", lambda e, hh=hh: e.scalar_tensor_tensor(out=Dp[:, hh, 0, :], in0=dgs[:, hh, 0, :], scalar=c31[:, hh:hh + 1],
                                                                     in1=cnegT, op0=ALU.subtract, op1=ALU.add),
                     reads=[cB], writes=[cB])
                K.op("dve", lambda e, hh=hh: e.tensor_scalar(out=Dp[:, hh, 1, :], in0=dgs[:, hh, 1, :], scalar1=c31[:, hh:hh + 1],
                                                              scalar2=None, op0=ALU.subtract),
                     reads=[cB], writes=[cB])
            K.barrier()
            A.release(mB0)

            def compute_hT(hT, hTB, wk_, nbuf=2):
                xt = [A.alloc([128, DM], F32) for _ in range(nbuf)]
                xtB = [Buf(f"xt{q}") for q in range(nbuf)]
                for i in range(NT):
                    K.dma(xt[i % nbuf], xv[:, i, :], writes=[xtB[i % nbuf]])
                    normT_tile(xt[i % nbuf], xtB[i % nbuf], gmix_s, hT[:, :, i * 128:(i + 1) * 128], hTB, wk_)

            pj = {"k": 0}

            def proj_fm(wsrc, hT, hTB, dst, dstB, wst, wstB, wb, wbB, scale, pbanks):
                K.dma(wst, wsrc, writes=[wstB])
                K.op("pool", lambda e: e.tensor_copy(out=wb, in_=wst), reads=[wstB], writes=[wbB])
                for tg in range(8):
                    bk = pbanks[pj["k"] % len(pbanks)]
                    pj["k"] += 1
                    for c in range(8):
                        K.op("pe", lambda e, bk=bk, c=c, tg=tg: e.matmul(banks[bk][:, :], wb[:, c * 128:(c + 1) * 128],
                                                                         hT[:, c, tg * 512:(tg + 1) * 512], start=(c == 0), stop=(c == 7)),
                             reads=[wbB, hTB], writes=[bankB[bk]], signal=(c == 7))
                    K.op("act", lambda e, bk=bk, tg=tg: e.mul(out=dst[:, tg * 512:(tg + 1) * 512], in_=banks[bk][:, :], mul=float(scale)),
                         reads=[bankB[bk]], writes=[dstB])

            def attn_phases():
                mB = A.mark()
                qaT = A.alloc([128, 4, S], BF16)
                kaT2 = A.alloc([128, S], BF16)
                qiT = A.alloc([128, 2, S], BF16)
                kiT4 = A.alloc([128, S], BF16)
                vaug = A.alloc([128, NT, 65], BF16)
                wabs = A.alloc([128, NT, 8], F32)
                wsgn = A.alloc([128, NT, 8], F32)
                qaB, kaB, qiB, kiB, vaB, wiB = Buf("qaT"), Buf("kaT2"), Buf("qiT"), Buf("kiT4"), Buf("vaug"), Buf("wi")
                mB1 = A.mark()
                hT = A.alloc([128, 8, S], BF16)
                hTB = Buf("hT")
                wk1 = {"sq": A.alloc([128, DM], BF16), "ss": A.alloc([128, 1], F32), "rs": A.alloc([128, 1], F32),
                       "xn": A.alloc([128, DM], BF16), "B": Buf("nwk1"), "bank": 0}
                compute_hT(hT, hTB, wk1)
                wst = A.alloc([128, 1024], F32)
                wb = [A.alloc([128, 1024], BF16) for _ in range(2)]
                wstB, wbB = Buf("wst"), [Buf("wb0"), Buf("wb1")]
                dsts = [(qaT[:, 0, :], qaB, 0.125), (qaT[:, 1, :], qaB, 0.125), (qaT[:, 2, :], qaB, 0.125), (qaT[:, 3, :], qaB, 0.125),
                        (kaT2, kaB, 1.0), (qiT[:, 0, :], qiB, 1.0), (qiT[:, 1, :], qiB, 1.0), (kiT4, kiB, 1.0)]
                for b_, (dst, dB, sc) in enumerate(dsts):
                    proj_fm(wfa[b_], hT, hTB, dst, dB, wst, wstB, wb[b_ % 2], wbB[b_ % 2], sc, [1, 2])
                wtf = A.alloc([128, 8 * 72], F32)
                wtb_ = A.alloc([128, 8, 72], BF16)
                wtB = Buf("wta")
                K.dma(wtf, wta, writes=[wtB])
                K.op("pool", lambda e: e.tensor_copy(out=wtb_, in_=wtf.rearrange("p (a b) -> p a b", b=72)), reads=[wtB], writes=[wtB])
                K.op("pool", lambda e: e.memset(vaug[:, :, 64:65], 1.0), writes=[vaB])
                for i in range(NT):
                    bk = [3, 4][i % 2]
                    for c in range(8):
                        K.op("pe", lambda e, bk=bk, c=c, i=i: e.matmul(banks[bk][:, 0:72], hT[:, c, i * 128:(i + 1) * 128], wtb_[:, c, :],
                                                                       start=(c == 0), stop=(c == 7)),
                             reads=[hTB, wtB], writes=[bankB[bk]], signal=(c == 7))
                    K.op("act", lambda e, bk=bk, i=i: e.activation(out=vaug[:, i, 0:64], in_=banks[bk][:, 0:64], func=AF.Copy),
                         reads=[bankB[bk]], writes=[vaB])
                    K.op("act", lambda e, bk=bk, i=i: e.activation(out=wabs[:, i, :], in_=banks[bk][:, 64:72], func=AF.Abs),
                         reads=[bankB[bk]], writes=[wiB])
                    K.op("act", lambda e, bk=bk, i=i: e.activation(out=wsgn[:, i, :], in_=banks[bk][:, 64:72], func=AF.Sign),
                         reads=[bankB[bk]], writes=[wiB])
                K.barrier()
                A.release(mB1)

                score = A.alloc([128, S], F32)
                scB = Buf("score")
                junk = A.alloc([128, S], U8)
                jB = Buf("junk")
                mneg = [A.alloc([128, S], BF16) for _ in range(2)]
                mnB = [Buf("mneg0"), Buf("mneg1")]
                mnegT2 = [A.alloc([128, NT, 512], BF16) for _ in range(2)]
                mtB2 = [Buf("mnegT0"), Buf("mnegT1")]
                rbuf = [A.alloc([128, 512], BF16) for _ in range(3)]
                rbB = [Buf("rb0"), Buf("rb1"), Buf("rb2")]
                dsg = [A.alloc([128, 8, 128], BF16) for _ in range(2)]
                dsgB = [Buf("dsg0"), Buf("dsg1")]
                pT = [A.alloc([128, 512], BF16) for _ in range(4)]
                pTB = [Buf(f"pT{i}") for i in range(4)]
                bs = A.alloc([128, 16], F32)
                stepv = A.alloc([128, NIT], F32)
                bsB = Buf("bs")
                rinv = A.alloc([128, 4], F32)
                rvB = Buf("rinv")
                oa_st = [A.alloc([128, 4, 512], BF16) for _ in range(2)]
                oaB = [Buf("oast0"), Buf("oast1")]
                if DEBUG:
                    dbv = dbg_oa.rearrange("(n p) d -> p n d", p=128)
                AT = {"pT": pT, "pTB": pTB, "rinv": rinv, "rvB": rvB, "abanks": [5, 6], "sbanks": [3, 4]}
                WSCALE = (8 ** -0.5) * (32 ** -0.5)
                cnt = {"ix": 0, "r": 0, "sc": 0, "p": 0, "acc": 0, "tr": 0}

                BG = {"gens": [], "rate": 1, "acc": 0.0}

                def bg_step():
                    BG["acc"] += BG["rate"]
                    while BG["acc"] >= 1.0 and BG["gens"]:
                        BG["acc"] -= 1.0
                        try:
                            next(BG["gens"][0])
                        except StopIteration:
                            BG["gens"].pop(0)
                    if not BG["gens"]:
                        BG["acc"] = 0.0

                def bg_drain():
                    while BG["gens"]:
                        try:
                            next(BG["gens"][0])
                        except StopIteration:
                            BG["gens"].pop(0)
                    BG["acc"] = 0.0

                def dsa_indexer(i):
                    Wd = 128 * (i + 1)
                    nch = (Wd + 511) // 512
                    dq = i % 2
                    K.op("dve", lambda e: e.tensor_tensor(out=dsg[dq], in0=ident.unsqueeze(1).to_broadcast([128, 8, 128]),
                                                          in1=wsgn[:, i, :].unsqueeze(2).to_broadcast([128, 8, 128]), op=ALU.mult),
                         reads=[cB, wiB], writes=[dsgB[dq]])

                    def s_mm(ch, cols, h):
                        bk = [1, 2][cnt["ix"] % 2]
                        cnt["ix"] += 1
                        pb = 32 * (h % 4)
                        K.op("pe", lambda e: e.matmul(
                            banks[bk][:, 0:cols], qiT[pb:pb + 32, h // 4, i * 128:(i + 1) * 128],
                            kiT4[pb:pb + 32, ch * 512:ch * 512 + cols], start=True, stop=True, tile_position=(pb, 0)),
                            reads=[qiB, kiB], writes=[bankB[bk]])
                        return bk

                    for ch in range(nch):
                        cols = min(512, Wd - 512 * ch)
                        nxt = s_mm(ch, cols, 0)
                        for h in range(8):
                            bk = nxt
                            if h + 1 < 8:
                                nxt = s_mm(ch, cols, h + 1)
                            r_ = cnt["r"] % 3
                            cnt["r"] += 1
                            K.op("act", lambda e, bk=bk, r_=r_, h=h, cols=cols: e.activation(
                                out=rbuf[r_][:, 0:cols], in_=banks[bk][:, 0:cols], func=AF.Relu, scale=wabs[:, i, h:h + 1]),
                                reads=[bankB[bk], wiB], writes=[rbB[r_]])
                            K.op("pe", lambda e, r_=r_, h=h, cols=cols: e.matmul(banks[0][:, 0:cols], dsg[dq][:, h, :], rbuf[r_][:, 0:cols],
                                                                               start=(h == 0), stop=(h == 7)),
                                 reads=[dsgB[dq], rbB[r_]], writes=[bankB[0]], signal=(h == 7))
                            yield
                        K.op("act", lambda e, ch=ch, cols=cols: e.activation(out=score[:, ch * 512:ch * 512 + cols], in_=banks[0][:, 0:cols], func=AF.Copy),
                             reads=[bankB[0]], writes=[scB])
                    sw = score[:, 0:Wd]
                    K.op("dve", lambda e: e.tensor_reduce(out=bs[:, 0:1], in_=sw, axis=AX.X, op=ALU.min), reads=[scB], writes=[bsB])
                    K.op("dve", lambda e: e.tensor_tensor(out=score[:, i * 128:(i + 1) * 128], in0=score[:, i * 128:(i + 1) * 128],
                                                          in1=ctok, op=ALU.add), reads=[scB, cB], writes=[scB])
                    K.op("dve", lambda e: e.tensor_reduce(out=bs[:, 1:2], in_=sw, axis=AX.X, op=ALU.max), reads=[scB], writes=[bsB])
                    K.op("dve", lambda e: e.tensor_tensor(out=bs[:, 2:3], in0=bs[:, 1:2], in1=bs[:, 0:1], op=ALU.subtract),
                         reads=[bsB], writes=[bsB])
                    K.op("dve", lambda e: e.tensor_scalar(out=bs[:, 2:3], in0=bs[:, 2:3], scalar1=1.0, scalar2=0.5, op0=ALU.add, op1=ALU.mult),
                         reads=[bsB], writes=[bsB])
                    K.op("dve", lambda e: e.scalar_tensor_tensor(out=bs[:, 3:4], in0=bs[:, 0:1], scalar=-1.0, in1=bs[:, 2:3],
                                                                 op0=ALU.add, op1=ALU.add), reads=[bsB], writes=[bsB])
                    K.op("dve", lambda e: e.tensor_scalar(out=stepv, in0=pow2, scalar1=bs[:, 2:3], scalar2=None, op0=ALU.mult),
                         reads=[bsB, cB], writes=[bsB])
                    yield
                    for k in range(NIT):
                        K.op("dve", lambda e: e.tensor_scalar(out=junk[:, 0:Wd], in0=sw, scalar1=bs[:, 3:4], scalar2=0.0,
                                                              op0=ALU.is_gt, op1=ALU.add, accum_out=bs[:, 4:5]),
                             reads=[scB, bsB], writes=[jB, bsB])
                        K.op("dve", lambda e: e.tensor_scalar(out=bs[:, 5:6], in0=bs[:, 4:5], scalar1=256.0, scalar2=-0.5,
                                                              op0=ALU.is_ge, op1=ALU.add), reads=[bsB], writes=[bsB])
                        K.op("dve", lambda e, k=k: e.scalar_tensor_tensor(out=bs[:, 3:4], in0=bs[:, 5:6], scalar=stepv[:, k:k + 1],
                                                                          in1=bs[:, 3:4], op0=ALU.mult, op1=ALU.add),
                             reads=[bsB], writes=[bsB])
                        yield
                    m_ = i % 2
                    K.op("dve", lambda e: e.tensor_tensor(out=bs[:, 3:4], in0=bs[:, 3:4], in1=stepv[:, NIT - 1:NIT], op=ALU.subtract),
                         reads=[bsB], writes=[bsB])
                    K.op("dve", lambda e, m_=m_: e.tensor_scalar(out=mneg[m_][:, 0:Wd], in0=sw, scalar1=bs[:, 3:4], scalar2=NEG,
                                                                 op0=ALU.is_le, op1=ALU.mult), reads=[scB, bsB], writes=[mnB[m_]])
                    tl = i % 4
                    j0 = 0
                    while j0 <= i:
                        nb = min(8, i + 1 - j0)
                        bk = 7
                        pv = banks[bk][:, 0:512].bitcast(BF16)
                        for jj in range(nb):
                            j = j0 + jj
                            K.op("pe", lambda e, j=j, jj=jj, m_=m_: e.transpose(pv[:, jj * 128:(jj + 1) * 128],
                                                                                 mneg[m_][:, j * 128:(j + 1) * 128], ident),
                                 reads=[mnB[m_], cB], writes=[bankB[bk]], signal=(jj == nb - 1))
                        K.op("act", lambda e, j0=j0, nb=nb, tl=tl: e.activation(
                            out=mnegT2[(i // 4) % 2][:, j0:j0 + nb, tl * 128:(tl + 1) * 128],
                            in_=pv[:, 0:nb * 128].rearrange("p (a b) -> p a b", b=128), func=AF.Copy),
                            reads=[bankB[bk]], writes=[mtB2[(i // 4) % 2]])
                        j0 += nb
                        yield

                def attention(T, h, hh, qT, qB_, kT, kB_, vA, vB_, jlist, maskT, maskB, kinds, writer, span=1000):
                    pb = 64 * (h % 2)
                    pT, pTB = AT["pT"], AT["pTB"]
                    npT = len(pT)
                    abanks = AT["abanks"]
                    sbanks = AT["sbanks"]
                    ab = abanks[cnt["acc"] % len(abanks)]
                    cnt["acc"] += 1
                    K.op("act", lambda e, ab=ab: e.activation(out=banks[ab][:, 0:260], in_=zeros_sb[:, 0:260], func=AF.Copy),
                         reads=[cB], writes=[bankB[ab]])

                    def stageA(j):
                        i_min = max(j, 4 * T)
                        i_max = min(4 * T + 3, j + span)
                        col0 = (i_min - 4 * T) * 128
                        col1 = (i_max - 4 * T + 1) * 128
                        bk = sbanks[cnt["sc"] % len(sbanks)]
                        cnt["sc"] += 1
                        extra = []
                        mt = maskT(j)
                        if mt is not None:
                            extra.append(("m", mt))
                        for i in range(i_min, i_max + 1):
                            kd = kinds(j, i)
                            if kd is not None:
                                extra.append(("b", i, kd))
                        K.op("pe", lambda e, bk=bk, j=j, col0=col0, col1=col1, ne=len(extra): e.matmul(
                            banks[bk][:, col0:col1], kT[pb:pb + 64, j * 128:(j + 1) * 128],
                            qT[pb:pb + 64, T * 512 + col0:T * 512 + col1], start=True, stop=(ne == 0)),
                            reads=[kB_, qB_], writes=[bankB[bk]], signal=(len(extra) == 0))
                        for n_, ex in enumerate(extra):
                            last = (n_ == len(extra) - 1)
                            if ex[0] == "m":
                                ml, mr = ex[1] if isinstance(ex[1], tuple) else (ident, ex[1])
                                K.op("pe", lambda e, bk=bk, col0=col0, col1=col1, ml=ml, mr=mr, last=last: e.matmul(
                                    banks[bk][:, col0:col1], ml, mr[:, col0:col1], start=False, stop=last),
                                    reads=[maskB, cB], writes=[bankB[bk]], signal=last)
                            else:
                                _, i, kd = ex
                                c0 = (i - 4 * T) * 128
                                K.op("pe", lambda e, bk=bk, c0=c0, kd=kd, last=last: e.matmul(
                                    banks[bk][:, c0:c0 + 128], ident, kd, start=False, stop=last),
                                    reads=[cB], writes=[bankB[bk]], signal=last)
                        return (j, bk, col0, col1, i_min, i_max)

                    def stageB(info):
                        j, bk, col0, col1, i_min, i_max = info
                        p_ = cnt["p"] % npT
                        cnt["p"] += 1
                        K.op("act", lambda e, bk=bk, p_=p_, col0=col0, col1=col1: e.activation(
                            out=pT[p_][:, col0:col1], in_=banks[bk][:, col0:col1], func=AF.Exp, bias=c31[:, hh:hh + 1]),
                            reads=[bankB[bk], cB], writes=[pTB[p_]])
                        return p_

                    def stageC(info, p_):
                        j, bk, col0, col1, i_min, i_max = info
                        ntl = i_max + 1 - i_min
                        for n_, i in enumerate(range(i_min, i_max + 1)):
                            tl = i - 4 * T
                            K.op("pe", lambda e, ab=ab, p_=p_, tl=tl, j=j: e.matmul(
                                banks[ab][:, tl * 65:(tl + 1) * 65], pT[p_][:, tl * 128:(tl + 1) * 128], vA[:, j, :],
                                start=False, stop=False, skip_group_check=True),
                                reads=[pTB[p_], vB_], writes=[bankB[ab]], signal=(n_ == ntl - 1))

                    prev = None
                    for j in jlist:
                        info = stageA(j)
                        if prev is not None:
                            stageC(*prev)
                        p_ = stageB(info)
                        prev = (info, p_)
                        bg_step()
                    stageC(*prev)
                    writer(ab)

                def dsa_head(T, h):
                    def writer(ab):
                        accv = banks[ab][:, 0:260].rearrange("p (a b) -> p a b", b=65)
                        K.op("dve", lambda e: e.reciprocal(out=rinv, in_=accv[:, :, 64]), reads=[bankB[ab]], writes=[rvB])
                        for tl in range(4):
                            K.op("dve", lambda e, tl=tl: e.tensor_scalar(out=oa_st[T % 2][:, tl, h * 64:(h + 1) * 64], in0=accv[:, tl, 0:64],
                                                                          scalar1=rinv[:, tl:tl + 1], scalar2=None, op0=ALU.mult),
                                 reads=[bankB[ab], rvB], writes=[oaB[T % 2]])

                    def kinds(j, i):
                        if i == j:
                            return Dp[:, h, 0, :]
                        if i == j + 1:
                            return Dp[:, h, 1, :]
                        return None
                    mT = mnegT2[T % 2]
                    attention(T, h, h, qaT[:, h // 2, :], qaB, kaT2, kaB, vaug, vaB, list(range(4 * T + 4)),
                              lambda j: mT[:, j, :], mtB2[T % 2], kinds, writer)

                def idx_steps(i):
                    return ((i + 1 + 3) // 4) * 8 + 1 + NIT + (i + 8) // 8

                BG["gens"] = [dsa_indexer(tl) for tl in range(4)]
                bg_drain()
                for T in range(8):
                    if T < 7:
                        BG["gens"] = [dsa_indexer(4 * (T + 1) + tl) for tl in range(4)]
                        nsteps = sum(idx_steps(4 * (T + 1) + tl) for tl in range(4)) + 8
                        BG["rate"] = nsteps / float(8 * (4 * T + 4) - 4)
                    for h in range(8):
                        dsa_head(T, h)
                    bg_drain()
                    K.dma(oa_scr[:, 4 * T:4 * T + 4, :], oa_st[T % 2], reads=[oaB[T % 2]])
                    if DEBUG:
                        K.dma(dbv[:, 4 * T:4 * T + 4, :], oa_st[T % 2], reads=[oaB[T % 2]])
                K.barrier()
                A.release(mB)
                def nsa_phase():
                    wfb = din("wfb", [10, 128, 1024])
                    wtbd = din("wtb", [128, 8 * 280])
                    bcgd = din("bcg", [128, 8 * 503])
                    bcmd = din("bcm", [128, 503])
                    ovd = din("ov", [128, 2 * 64])
                    m0d = din("selm0", [128, 126])
                    m1d = din("selm1", [128, 126])
                    wfard = din("wfar", [128, 128])
                    eexpd = din("eexp", [64, S])
                    w1kd = din("w1k", [128, 32 * 128])
                    w1vd = din("w1v", [128, 32 * 128])
                    w2kd = din("w2k", [128, 128])
                    w2vd = din("w2v", [128, 64])
                    pekd = din("pek", [128, 32])
                    pevd = din("pev", [128, 32])
                    mC = A.mark()
                    Bc = A.alloc([128, 8, 503], BF16)
                    ov = A.alloc([128, 2, 64], BF16)
                    selm0 = A.alloc([128, 126], F32)
                    selm1 = A.alloc([128, 126], F32)
                    wfar = A.alloc([128, 128], BF16)
                    ncB = Buf("nsac")
                    qbT = A.alloc([128, 4, S], BF16)
                    ksT4 = A.alloc([128, 2, S], BF16)
                    kwT4 = A.alloc([128, 2, S], BF16)
                    vaug_s = A.alloc([128, NT, 2, 65], BF16)
                    vaug_w = A.alloc([128, NT, 2, 65], BF16)
                    gates = A.alloc([128, NT, 24], F32)
                    kcmpT = A.alloc([128, 2, 256], BF16)
                    vcmp = A.alloc([128, 2, 2, 64], BF16)
                    qbB, ksB, kwB, vsB, vwB, gtB, kcB, vcB = (Buf(n) for n in ("qbT", "ksT4", "kwT4", "vaug_s", "vaug_w", "gates", "kcmpT", "vcmp"))
                    mC1 = A.mark()
                    t_bcg = A.alloc([128, 8, 503], F32)
                    t_bcm = A.alloc([128, 503], F32)
                    t_ov = A.alloc([128, 128], F32)
                    t_wf = A.alloc([128, 128], F32)
                    K.dma(t_bcg, bcgd.rearrange("p (a b) -> p a b", b=503), writes=[ncB])
                    K.dma(t_bcm, bcmd, writes=[ncB])
                    K.dma(t_ov, ovd, writes=[ncB])
                    K.dma(t_wf, wfard, writes=[ncB])
                    K.dma(selm0, m0d, writes=[ncB])
                    K.dma(selm1, m1d, writes=[ncB])
                    K.op("dve", lambda e: e.tensor_tensor(out=Bc, in0=t_bcg, in1=t_bcm.unsqueeze(1).to_broadcast([128, 8, 503]), op=ALU.add),
                         reads=[ncB], writes=[ncB])
                    K.op("dve", lambda e: e.tensor_copy(out=ov, in_=t_ov.rearrange("p (a b) -> p a b", b=64)), reads=[ncB], writes=[ncB])
                    K.op("dve", lambda e: e.tensor_copy(out=wfar, in_=t_wf), reads=[ncB], writes=[ncB])
                    K.barrier()
                    A.release(mC1)
                    kcT = A.alloc([128, S], BF16)
                    vcT = A.alloc([128, S], BF16)
                    kctB, vctB = Buf("kcT"), Buf("vcT")
                    wtb_ = A.alloc([128, 8, 280], BF16)
                    wst = A.alloc([128, 1024], F32)
                    wb = [A.alloc([128, 1024], BF16)] * 2
                    wstB, wbB = Buf("wst2"), [Buf("wb20")] * 2
                    mC2 = A.mark()
                    wtf = A.alloc([128, 8 * 280], F32)
                    wtB = Buf("wtb")
                    K.dma(wtf, wtbd, writes=[wtB])
                    K.op("pool", lambda e: e.tensor_copy(out=wtb_, in_=wtf.rearrange("p (a b) -> p a b", b=280)), reads=[wtB], writes=[wtB])
                    K.barrier()
                    A.release(mC2)
                    hT = A.alloc([128, 8, S], BF16)
                    hTB = Buf("hT2")
                    mC3 = A.mark()
                    wk2 = {"sq": A.alloc([128, DM], BF16), "ss": A.alloc([128, 1], F32), "rs": A.alloc([128, 1], F32),
                           "xn": A.alloc([128, DM], BF16), "B": Buf("nwk2"), "bank": 0}
                    compute_hT(hT, hTB, wk2, nbuf=1)
                    K.barrier()
                    A.release(mC3)
                    dsts = [(qbT[:, b_, :], qbB, 0.125) for b_ in range(4)] + \
                           [(ksT4[:, 0, :], ksB, 1.0), (ksT4[:, 1, :], ksB, 1.0), (kwT4[:, 0, :], kwB, 1.0), (kwT4[:, 1, :], kwB, 1.0),
                            (kcT, kctB, 1.0), (vcT, vctB, 1.0)]
                    for b_, (dst, dB, sc) in enumerate(dsts):
                        proj_fm(wfb[b_], hT, hTB, dst, dB, wst, wstB, wb[b_ % 2], wbB[b_ % 2], sc, [1, 2])
                    K.op("pool", lambda e: e.memset(vaug_s[:, :, :, 64:65], 1.0), writes=[vsB])
                    K.op("pool", lambda e: e.memset(vaug_w[:, :, :, 64:65], 1.0), writes=[vwB])
                    for i in range(NT):
                        bk = [3, 4][i % 2]
                        for c in range(8):
                            K.op("pe", lambda e, bk=bk, c=c, i=i: e.matmul(banks[bk][:, 0:280], hT[:, c, i * 128:(i + 1) * 128], wtb_[:, c, :],
                                                                           start=(c == 0), stop=(c == 7)),
                                 reads=[hTB, wtB], writes=[bankB[bk]], signal=(c == 7))
                        K.op("act", lambda e, bk=bk, i=i: e.activation(out=vaug_s[:, i, :, 0:64],
                                                                        in_=banks[bk][:, 0:128].rearrange("p (a b) -> p a b", b=64), func=AF.Copy),
                             reads=[bankB[bk]], writes=[vsB])
                        K.op("act", lambda e, bk=bk, i=i: e.activation(out=vaug_w[:, i, :, 0:64],
                                                                        in_=banks[bk][:, 128:256].rearrange("p (a b) -> p a b", b=64), func=AF.Copy),
                             reads=[bankB[bk]], writes=[vwB])
                        K.op("act", lambda e, bk=bk, i=i: e.activation(out=gates[:, i, :], in_=banks[bk][:, 256:280], func=AF.Sigmoid),
                             reads=[bankB[bk]], writes=[gtB])
                    K.barrier()
                    A.release(mC2)
                    w1f = A.alloc([128, 32, 128], F32)
                    w1b = A.alloc([128, 32, 128], BF16)
                    w2f = A.alloc([128, 128], F32)
                    w2b = A.alloc([128, 128], BF16)
                    pef = A.alloc([128, 32], F32)
                    peb = A.alloc([128, 32], BF16)
                    b1 = A.alloc([128, 1], F32)
                    zt_ = A.alloc([128, 256], F32)
                    z2_ = A.alloc([128, 256], F32)
                    gT_ = A.alloc([128, 256], BF16)
                    cwB = Buf("cmpw")
                    czB = Buf("cmpz")
                    for kind in range(2):
                        xcT, xB_ = (kcT, kctB) if kind == 0 else (vcT, vctB)
                        K.dma(w1f, (w1kd if kind == 0 else w1vd).rearrange("p (a b) -> p a b", b=128), writes=[cwB])
                        K.dma(pef, pekd if kind == 0 else pevd, writes=[cwB])
                        if kind == 0:
                            K.dma(w2f, w2kd, writes=[cwB])
                        else:
                            K.dma(w2f[:, 0:64], w2vd, writes=[cwB])
                        K.op("pool", lambda e: e.tensor_copy(out=w1b, in_=w1f), reads=[cwB], writes=[cwB])
                        K.op("pool", lambda e: e.tensor_copy(out=w2b, in_=w2f), reads=[cwB], writes=[cwB])
                        K.op("pool", lambda e: e.tensor_copy(out=peb, in_=pef), reads=[cwB], writes=[cwB])
                        for l in range(32):
                            K.op("pe", lambda e, l=l: e.matmul(banks[5][:, 0:1], w1b[0:64, l, :], peb[0:64, l:l + 1], start=(l == 0), stop=(l == 31)),
                                 reads=[cwB], writes=[bankB[5]], signal=(l == 31))
                        K.op("act", lambda e: e.activation(out=b1, in_=banks[5][:, 0:1], func=AF.Copy), reads=[bankB[5]], writes=[czB])
                        for g in range(2):
                            pb = 64 * g
                            for l in range(32):
                                K.op("pe", lambda e, l=l, pb=pb, xcT=xcT: e.matmul(banks[6][:, 0:255], w1b[pb:pb + 64, l, :],
                                                                                  xcT.rearrange("p (c s) -> p c s", s=16)[pb:pb + 64, (l // 16):(l // 16) + 255, l % 16], start=(l == 0), stop=(l == 31)),
                                     reads=[cwB, xB_], writes=[bankB[6]], signal=(l == 31))
                            zz, z2, gT = zt_[:, 0:255], z2_[:, 0:255], gT_[:, 0:255]
                            K.op("act", lambda e: e.activation(out=zz, in_=banks[6][:, 0:255], func=AF.Identity, bias=b1), reads=[bankB[6], czB], writes=[czB])
                            K.op("dve", lambda e: e.tensor_tensor(out=z2, in0=zz, in1=zz, op=ALU.mult), reads=[czB], writes=[czB])
                            K.op("dve", lambda e: e.tensor_scalar(out=z2, in0=z2, scalar1=0.044715, scalar2=1.0, op0=ALU.mult, op1=ALU.add), reads=[czB], writes=[czB])
                            K.op("dve", lambda e: e.tensor_tensor(out=z2, in0=z2, in1=zz, op=ALU.mult), reads=[czB], writes=[czB])
                            K.op("act", lambda e: e.activation(out=z2, in_=z2, func=AF.Sigmoid, scale=1.5957691216057308), reads=[czB], writes=[czB])
                            K.op("dve", lambda e: e.tensor_tensor(out=gT, in0=z2, in1=zz, op=ALU.mult), reads=[czB], writes=[czB])
                            if kind == 0:
                                K.op("pe", lambda e: e.matmul(banks[7][:, 0:255], w2b, gT, start=True, stop=True), reads=[cwB, czB], writes=[bankB[7]])
                                K.op("act", lambda e, g=g: e.activation(out=kcmpT[:, g, 0:255], in_=banks[7][:, 0:255], func=AF.Copy),
                                     reads=[bankB[7]], writes=[kcB])
                            else:
                                for ch in range(2):
                                    m_ = 128 if ch == 0 else 127
                                    K.op("pe", lambda e, ch=ch, m_=m_: e.matmul(banks[7][0:m_, 0:64], gT_[:, ch * 128:ch * 128 + m_], w2b[:, 0:64],
                                                                                start=True, stop=True), reads=[cwB, czB], writes=[bankB[7]])
                                    K.op("act", lambda e, ch=ch, m_=m_, g=g: e.activation(out=vcmp[0:m_, ch, g, :], in_=banks[7][0:m_, 0:64], func=AF.Copy),
                                         reads=[bankB[7]], writes=[vcB])
                    K.barrier()
                    A.release(mC1)
                    Ebuf = [A.alloc([128, 256], F32) for _ in range(2)]
                    Pbuf = [A.alloc([128, 256], BF16) for _ in range(2)]
                    PTs = [A.alloc([128, 2, 128], BF16) for _ in range(2)]
                    PTB = [Buf("PT0"), Buf("PT1")]
                    EB, PB_ = [Buf("E0"), Buf("E1")], [Buf("P0"), Buf("P1")]
                    cs = [A.alloc([128, 8], F32) for _ in range(2)]
                    csB = [Buf("cs0"), Buf("cs1")]
                    scS = A.alloc([128, 64], F32)
                    scW = A.alloc([128, 64], F32)
                    m8 = A.alloc([128, 16], F32)
                    ssB = Buf("selsc")
                    seln = [A.alloc([128, 4, 2, 64], BF16) for _ in range(2)]
                    slB = [Buf("seln0"), Buf("seln1")]
                    Eexp = A.alloc([128, S], BF16)
                    selT = [A.alloc([128, 512], BF16) for _ in range(2)]
                    stB_ = [Buf("selT0"), Buf("selT1")]
                    mtB = Buf("unused_mask")
                    t_ee = A.alloc([128, 2048], F32)
                    K.op("pool", lambda e: e.memset(Eexp, 0.0), writes=[ncB])
                    K.op("pool", lambda e: e.memset(selT[0], 0.0), writes=[stB_[0]])
                    K.op("pool", lambda e: e.memset(selT[1], 0.0), writes=[stB_[1]])
                    for hf in range(2):
                        K.dma(t_ee[0:64, :], eexpd[:, hf * 2048:(hf + 1) * 2048], writes=[ncB])
                        K.op("pool", lambda e, hf=hf: e.tensor_copy(out=Eexp[0:64, hf * 2048:(hf + 1) * 2048], in_=t_ee[0:64, :]), reads=[ncB], writes=[ncB])
                    K.barrier()
                    ob_st = [A.alloc([128, 4, 512], F32) for _ in range(2)]
                    obB = [Buf("obst0"), Buf("obst1")]
                    ob_bf = A.alloc([128, 4, 512], BF16)
                    obfB = Buf("obbf")
                    coef = A.alloc([128, 4], F32)
                    cfB = Buf("coef")
                    AT["pT"] = [A.alloc([128, 512], BF16) for _ in range(4)]
                    AT["pTB"] = [Buf(f"pTn{q}") for q in range(4)]
                    AT["rinv"] = A.alloc([128, 4], F32)
                    AT["rvB"] = Buf("rinvn")
                    AT["abanks"] = [5, 6]
                    AT["sbanks"] = [3, 4]
                    if DEBUG:
                        dbv2 = dbg_ob.rearrange("(n p) d -> p n d", p=128)

                    def compressed_tile(i, tl, par):
                        for g in range(2):
                            K.op("dve", lambda e: e.memset(banks[0][:, 0:320], 0.0), writes=[bankB[0]])
                            yield
                            for r_ in range(4):
                                h = 4 * g + r_
                                q_ = h % 2
                                pb = 64 * (h % 2)
                                bk = [1, 2][h % 2]
                                csq, Eq, Pq = cs[q_], Ebuf[q_], Pbuf[q_]
                                K.op("pe", lambda e, bk=bk, h=h, pb=pb, g=g: e.matmul(banks[bk][:, 0:255], qbT[pb:pb + 64, h // 2, i * 128:(i + 1) * 128],
                                                                                     kcmpT[pb:pb + 64, g, 0:255], start=True, stop=False),
                                     reads=[qbB, kcB], writes=[bankB[bk]], signal=False)
                                K.op("pe", lambda e, bk=bk, h=h: e.matmul(banks[bk][:, 0:255], ident, Bc[:, h, 248 - 8 * i:248 - 8 * i + 255],
                                                                           start=False, stop=True), reads=[ncB, cB], writes=[bankB[bk]])
                                K.op("dve", lambda e, bk=bk, csq=csq: e.tensor_reduce(out=csq[:, 0:1], in_=banks[bk][:, 0:255], axis=AX.X, op=ALU.max),
                                     reads=[bankB[bk]], writes=[csB[q_]])
                                K.op("dve", lambda e, csq=csq: e.tensor_scalar(out=csq[:, 1:2], in0=csq[:, 0:1], scalar1=-1000.0, scalar2=-1.0,
                                                                               op0=ALU.max, op1=ALU.mult), reads=[csB[q_]], writes=[csB[q_]])
                                K.op("act", lambda e, bk=bk, csq=csq, Eq=Eq: e.activation(out=Eq[:, 0:255], in_=banks[bk][:, 0:255], func=AF.Exp, bias=csq[:, 1:2],
                                                                                         accum_out=csq[:, 2:3]), reads=[bankB[bk], csB[q_]], writes=[EB[q_], csB[q_]])
                                K.op("dve", lambda e, csq=csq: e.tensor_scalar(out=csq[:, 3:4], in0=csq[:, 2:3], scalar1=1e-30, scalar2=None, op0=ALU.max),
                                     reads=[csB[q_]], writes=[csB[q_]])
                                K.op("dve", lambda e, csq=csq: e.reciprocal(out=csq[:, 3:4], in_=csq[:, 3:4]), reads=[csB[q_]], writes=[csB[q_]])
                                K.op("dve", lambda e, csq=csq, Eq=Eq, Pq=Pq: e.tensor_scalar(out=Pq[:, 0:255], in0=Eq[:, 0:255], scalar1=csq[:, 3:4], scalar2=None, op0=ALU.mult),
                                     reads=[EB[q_], csB[q_]], writes=[PB_[q_]])
                                yield
                                yield
                                yield
                                pv = banks[7][:, 0:128].bitcast(BF16)
                                K.op("pe", lambda e, Pq=Pq, pv=pv: e.transpose(pv[:, 0:128], Pq[:, 0:128], ident), reads=[PB_[q_], cB], writes=[bankB[7]], signal=False)
                                K.op("pe", lambda e, Pq=Pq, pv=pv: e.transpose(pv[0:127, 128:256], Pq[:, 128:255], ident), reads=[PB_[q_], cB], writes=[bankB[7]])
                                pt = PTs[q_]
                                ptB = PTB[q_]
                                K.op("act", lambda e, pt=pt, pv=pv: e.activation(out=pt[:, 0, :], in_=pv[:, 0:128], func=AF.Copy), reads=[bankB[7]], writes=[ptB])
                                K.op("act", lambda e, pt=pt, pv=pv: e.activation(out=pt[0:127, 1, :], in_=pv[0:127, 128:256], func=AF.Copy), reads=[bankB[7]], writes=[ptB])
                                yield
                                yield
                                for ch in range(2):
                                    m_ = 128 if ch == 0 else 127
                                    K.op("pe", lambda e, pt=pt, ch=ch, m_=m_, r_=r_, g=g: e.matmul(banks[0][:, r_ * 64:(r_ + 1) * 64], pt[0:m_, ch, :], vcmp[0:m_, ch, g, :],
                                                                                                start=False, stop=False, skip_group_check=True),
                                         reads=[ptB, vcB], writes=[bankB[0]], signal=False)
                                for ch in range(2):
                                    m_ = 128 if ch == 0 else 127
                                    K.op("pe", lambda e, pt=pt, ch=ch, m_=m_: e.matmul(
                                        banks[0][:, 256:320], pt[0:m_, ch, :], ov[0:m_, ch, :], start=False, stop=False, skip_group_check=True),
                                        reads=[ptB, ncB], writes=[bankB[0]], signal=(ch == 1))
                                yield
                            for r_ in range(4):
                                h = 4 * g + r_
                                K.op("dve", lambda e, h=h, r_=r_: e.tensor_scalar(out=ob_st[par][:, tl, h * 64:(h + 1) * 64], in0=banks[0][:, r_ * 64:(r_ + 1) * 64],
                                                                                  scalar1=gates[:, i, h:h + 1], scalar2=None, op0=ALU.mult),
                                     reads=[bankB[0], gtB], writes=[obB[par]])
                            lo_ = 62 - 2 * i
                            K.op("dve", lambda e, lo_=lo_: e.tensor_tensor(out=scS, in0=banks[0][:, 256:320], in1=selm0[:, lo_:lo_ + 64], op=ALU.mult),
                                 reads=[bankB[0], ncB], writes=[ssB])
                            K.op("dve", lambda e, lo_=lo_: e.tensor_tensor(out=scS, in0=scS, in1=selm1[:, lo_:lo_ + 64], op=ALU.add), reads=[ssB, ncB], writes=[ssB])
                            K.op("dve", lambda e: e.memset(scS[:, 0:1], 30000.0), writes=[ssB])
                            K.op("dve", lambda e: e.max(out=m8[:, 0:8], in_=scS), reads=[ssB], writes=[ssB])
                            K.op("dve", lambda e: e.match_replace(out=scW, in_to_replace=m8[:, 0:8], in_values=scS, imm_value=-1e30), reads=[ssB], writes=[ssB])
                            K.op("dve", lambda e: e.max(out=m8[:, 8:16], in_=scW), reads=[ssB], writes=[ssB])
                            K.op("dve", lambda e, g=g: e.tensor_scalar(out=seln[par][:, tl, g, :], in0=scS, scalar1=m8[:, 15:16], scalar2=NEG, op0=ALU.is_lt, op1=ALU.mult),
                                 reads=[ssB], writes=[slB[par]])
                            yield

                    def nsa_writer_factory(T, h, gate_off):
                        par = T % 2

                        def writer(ab):
                            accv = banks[ab][:, 0:260].rearrange("p (a b) -> p a b", b=65)
                            rinv, rvB = AT["rinv"], AT["rvB"]
                            K.op("dve", lambda e: e.reciprocal(out=rinv, in_=accv[:, :, 64]), reads=[bankB[ab]], writes=[rvB])
                            K.op("dve", lambda e: e.tensor_tensor(out=coef, in0=rinv, in1=gates[:, 4 * T:4 * T + 4, gate_off + h], op=ALU.mult),
                                 reads=[rvB, gtB], writes=[cfB])
                            for tl in range(4):
                                K.op("dve", lambda e, tl=tl: e.scalar_tensor_tensor(out=ob_st[par][:, tl, h * 64:(h + 1) * 64], in0=accv[:, tl, 0:64],
                                                                                     scalar=coef[:, tl:tl + 1], in1=ob_st[par][:, tl, h * 64:(h + 1) * 64],
                                                                                     op0=ALU.mult, op1=ALU.add),
                                     reads=[bankB[ab], cfB, obB[par]], writes=[obB[par]])
                        return writer

                    def sel_prep(T, g):
                        par = T % 2
                        pv = banks[7][:, 0:256].bitcast(BF16)
                        for tl in range(4):
                            K.op("pe", lambda e, tl=tl: e.transpose(pv[0:64, tl * 128:(tl + 1) * 128], seln[par][:, tl, g, :], ident),
                                 reads=[slB[par], cB], writes=[bankB[7]], signal=(tl == 3))
                        K.op("act", lambda e: e.activation(out=selT[g][0:64, :], in_=pv[0:64, :], func=AF.Copy),
                             reads=[bankB[7]], writes=[stB_[g]])

                    def sel_head(T, h):
                        g = h // 4

                        def kinds(j, i):
                            if i == j:
                                return Dp[:, 8 + h, 0, :]
                            if i == j + 1:
                                return Dp[:, 8 + h, 1, :]
                            return None
                        attention(T, h, 8 + h, qbT[:, h // 2, :], qbB, ksT4[:, g, :], ksB, vaug_s[:, :, g, :], vsB,
                                  list(range(4 * T + 4)), lambda j: (Eexp[:, j * 128:(j + 1) * 128], selT[g]), stB_[g],
                                  kinds, nsa_writer_factory(T, h, 8))

                    def win_head(T, h):
                        g = h // 4

                        def kinds_w(j, i):
                            if i == j:
                                return Dp[:, 8 + h, 0, :]
                            if i == j + 1:
                                return Dp[:, 8 + h, 1, :]
                            if i == j + 4:
                                return wfar
                            return None
                        attention(T, h, 8 + h, qbT[:, h // 2, :], qbB, kwT4[:, g, :], kwB, vaug_w[:, :, g, :], vwB,
                                  list(range(max(0, 4 * T - 4), 4 * T + 4)), lambda j: None, mtB, kinds_w, nsa_writer_factory(T, h, 16), span=4)

                    BG["gens"] = [compressed_tile(tl, tl, 0) for tl in range(4)]
                    bg_drain()
                    for T in range(8):
                        par = T % 2
                        if T < 7:
                            BG["gens"] = [compressed_tile(4 * (T + 1) + tl, tl, (T + 1) % 2) for tl in range(4)]
                            nsteps = 4 * 2 * (2 + 4 * 7) + 8
                            niter = 8 * (4 * T + 4) + sum(min(8, 4 * T + 4) for _ in range(8))
                            BG["rate"] = nsteps / float(niter - 8)
                        for g in range(2):
                            sel_prep(T, g)
                            for r_ in range(4):
                                sel_head(T, 4 * g + r_)
                        for h in range(8):
                            win_head(T, h)
                        bg_drain()
                        K.op("pool", lambda e, par=par: e.tensor_copy(out=ob_bf, in_=ob_st[par]), reads=[obB[par]], writes=[obfB])
                        K.dma(ob_scr[:, 4 * T:4 * T + 4, :], ob_bf, reads=[obfB])
                        if DEBUG:
                            K.dma(dbv2[:, 4 * T:4 * T + 4, :], ob_st[par], reads=[obB[par]])
                    K.barrier()
                    A.release(mC)

                if stage >= 3:
                    nsa_phase()
                else:
                    zt = A.alloc([128, 4, 512], BF16)
                    zB = Buf("zt")
                    K.op("pool", lambda e: e.memset(zt, 0.0), writes=[zB])
                    for T in range(8):
                        K.dma(ob_scr[:, 4 * T:4 * T + 4, :], zt, reads=[zB])


            if stage in (2, 3):
                attn_phases()

            K.barrier()
            mD = A.mark()
            wg = A.alloc([128, 8, 2048], BF16)
            wba = A.alloc([128, 4, DM], BF16)
            wbb = A.alloc([128, 4, DM], BF16)
            wo = A.alloc([128, 8, DM], BF16)
            wDB = Buf("wD")
            mS2 = A.mark()
            stgD = [A.alloc([128, 2048], F32) for _ in range(2)]
            stgB = [Buf("stgD0"), Buf("stgD1")]
            k = 0
            jobs = []
            wgv = wgd.rearrange("(c p) n -> p c n", p=128)
            for c in range(8):
                jobs.append((wgv[:, c, :], wg[:, c, :], 2048))
            for (dr, sb_, nchk) in ((wbad, wba, 4), (wbbd, wbb, 4), (wod, wo, 8)):
                drv = dr.rearrange("(c p) n -> p c n", p=128)
                for c2 in range(nchk // 2):
                    jobs.append((drv[:, 2 * c2:2 * c2 + 2, :], sb_[:, 2 * c2:2 * c2 + 2, :], -1))
            for (src, dst, kind) in jobs:
                s_ = k % 2
                k += 1
                if kind == 2048:
                    K.dma(stgD[s_], src, writes=[stgB[s_]])
                    K.op("pool", lambda e, s_=s_, dst=dst: e.tensor_copy(out=dst, in_=stgD[s_]), reads=[stgB[s_]], writes=[wDB])
                else:
                    sv = stgD[s_].rearrange("p (a b) -> p a b", b=DM)
                    K.dma(sv, src, writes=[stgB[s_]])
                    K.op("pool", lambda e, sv=sv, dst=dst: e.tensor_copy(out=dst, in_=sv), reads=[stgB[s_]], writes=[wDB])
            K.barrier()
            A.release(mS2)
            xtD = [A.alloc([128, DM], F32) for _ in range(2)]
            xtDB = [Buf("xtD0"), Buf("xtD1")]
            hTt = [A.alloc([128, 8, 128], BF16) for _ in range(2)]
            hTtB = [Buf("hTt0"), Buf("hTt1")]
            wkD = {"sq": A.alloc([128, DM], BF16), "ss": A.alloc([128, 1], F32), "rs": A.alloc([128, 1], F32),
                   "xn": A.alloc([128, DM], BF16), "B": Buf("nwkD"), "bank": 0}
            oin = [A.alloc([128, 2, 512], BF16) for _ in range(2)]
            oinB = [Buf("oin0"), Buf("oin1")]
            oT = [A.alloc([128, 8, 128], BF16) for _ in range(2)]
            oTB = [Buf("oT0"), Buf("oT1")]
            sig = [A.alloc([128, 2048], BF16) for _ in range(2)]
            sigB = [Buf("sig0"), Buf("sig1")]
            ya = A.alloc([128, DM], F32)
            mixb = A.alloc([128, DM], BF16)
            mxB = Buf("mix")
            yaB = Buf("ya")
            mixT = A.alloc([128, 8, 128], BF16)
            mTB = Buf("mixT")
            x2o = [A.alloc([128, DM], F32) for _ in range(2)]
            x2oB = [Buf("x2o0"), Buf("x2o1")]
            if DEBUG:
                dbgf = A.alloc([128, 2048], F32)
                dbgB = Buf("dbgf")
            def d_tile(i):
                    a = i % 2
                    K.dma(xtD[a], xv[:, i, :], writes=[xtDB[a]])
                    normT_tile(xtD[a], xtDB[a], gmix_s, hTt[a], hTtB[a], wkD)
                    pv = banks[1][:, 0:512].bitcast(BF16)
                    K.dma(oin[a][:, 0, :], oa_scr[:, i, :], writes=[oinB[a]])
                    K.dma(oin[a][:, 1, :], ob_scr[:, i, :], writes=[oinB[a]])
                    for c in range(8):
                        src = oin[a][:, c // 4, (c % 4) * 128:(c % 4 + 1) * 128]
                        K.op("pe", lambda e, c=c, src=src: e.transpose(pv[:, c * 128:(c + 1) * 128], src, ident),
                             reads=[oinB[a], cB], writes=[bankB[1]], signal=(c == 7))
                    K.op("act", lambda e: e.activation(out=oT[a], in_=pv.rearrange("p (a b) -> p a b", b=128), func=AF.Copy),
                         reads=[bankB[1]], writes=[oTB[a]])
                    for q in range(4):
                        bk = [2, 3][q % 2]
                        for c in range(8):
                            K.op("pe", lambda e, bk=bk, c=c, q=q: e.matmul(banks[bk][:, :], hTt[a][:, c, :], wg[:, c, q * 512:(q + 1) * 512],
                                                                           start=(c == 0), stop=(c == 7)),
                                 reads=[hTtB[a], wDB], writes=[bankB[bk]], signal=(c == 7))
                        if DEBUG and i == 0 and q == 0:
                            K.op("act", lambda e, bk=bk: e.activation(out=dbgf[:, 0:512], in_=banks[bk][:, :], func=AF.Copy), reads=[bankB[bk]], writes=[dbgB])
                            K.dma(dbg_z, dbgf[:, 0:512], reads=[dbgB])
                            K.op("pool", lambda e: e.tensor_copy(out=dbgf[:, 0:1024], in_=hTt[a].rearrange("p a b -> p (a b)")), reads=[hTtB[a]], writes=[dbgB])
                            K.dma(dbg_hT, dbgf[:, 0:1024], reads=[dbgB])
                            K.op("pool", lambda e: e.tensor_copy(out=dbgf, in_=wg[:, 0, :]), reads=[wDB], writes=[dbgB])
                            K.dma(dbg_wg, dbgf, reads=[dbgB])
                        K.op("act", lambda e, bk=bk, q=q: e.activation(out=sig[a][:, q * 512:(q + 1) * 512], in_=banks[bk][:, :], func=AF.Sigmoid),
                             reads=[bankB[bk]], writes=[sigB[a]])
                    yield
                    for br in range(2):
                        wbr = wba if br == 0 else wbb
                        for nh in range(2):
                            bk = [4, 5][nh]
                            for c in range(4):
                                K.op("pe", lambda e, bk=bk, c=c, br=br, nh=nh, wbr=wbr: e.matmul(
                                    banks[bk][:, :], oT[a][:, br * 4 + c, :], wbr[:, c, nh * 512:(nh + 1) * 512], start=(c == 0), stop=(c == 3)),
                                    reads=[oTB[a], wDB], writes=[bankB[bk]], signal=(c == 3))
                            if br == 0:
                                K.op("dve", lambda e, bk=bk, nh=nh: e.tensor_tensor(out=ya[:, nh * 512:(nh + 1) * 512], in0=banks[bk][:, :],
                                                                                     in1=sig[a][:, nh * 512:(nh + 1) * 512], op=ALU.mult),
                                     reads=[bankB[bk], sigB[a]], writes=[yaB])
                            else:
                                K.op("dve", lambda e, bk=bk, nh=nh: e.tensor_tensor(out=banks[bk][:, :], in0=banks[bk][:, :],
                                                                                     in1=sig[a][:, 1024 + nh * 512:1024 + (nh + 1) * 512], op=ALU.mult),
                                     reads=[bankB[bk], sigB[a]], writes=[bankB[bk]])
                                K.op("dve", lambda e, bk=bk, nh=nh: e.tensor_tensor(out=mixb[:, nh * 512:(nh + 1) * 512], in0=banks[bk][:, :],
                                                                                     in1=ya[:, nh * 512:(nh + 1) * 512], op=ALU.add),
                                     reads=[bankB[bk], yaB], writes=[mxB])
                    pv6 = banks[6][:, 0:512].bitcast(BF16)
                    for c in range(8):
                        K.op("pe", lambda e, c=c: e.transpose(pv6[:, c * 128:(c + 1) * 128], mixb[:, c * 128:(c + 1) * 128], ident),
                             reads=[mxB, cB], writes=[bankB[6]], signal=(c == 7))
                    K.op("act", lambda e: e.activation(out=mixT, in_=pv6.rearrange("p (a b) -> p a b", b=128), func=AF.Copy),
                         reads=[bankB[6]], writes=[mTB])
                    for nh in range(2):
                        bk = [7, 4][nh]
                        for c in range(8):
                            K.op("pe", lambda e, bk=bk, c=c, nh=nh: e.matmul(banks[bk][:, :], mixT[:, c, :], wo[:, c, nh * 512:(nh + 1) * 512],
                                                                             start=(c == 0), stop=(c == 7)),
                                 reads=[mTB, wDB], writes=[bankB[bk]], signal=(c == 7))
                        K.op("dve", lambda e, bk=bk, nh=nh, a=a: e.tensor_tensor(out=x2o[a][:, nh * 512:(nh + 1) * 512], in0=banks[bk][:, :],
                                                                                 in1=xtD[a][:, nh * 512:(nh + 1) * 512], op=ALU.add),
                             reads=[bankB[bk], xtDB[a]], writes=[x2oB[a]])
                    K.dma(x2v_w[:, i, :], x2o[a], reads=[x2oB[a]])
                    if DEBUG:
                        K.dma(dbg_x2.rearrange("(n p) d -> p n d", p=128)[:, i, :], x2o[a], reads=[x2oB[a]])
                        K.dma(dbg_ya.rearrange("(n p) d -> p n d", p=128)[:, i, :], ya, reads=[yaB])
                        K.op("pool", lambda e: e.tensor_copy(out=dbgf[:, 0:DM], in_=mixb), reads=[mxB], writes=[dbgB])
                        K.dma(dbg_mix.rearrange("(n p) d -> p n d", p=128)[:, i, :], dbgf[:, 0:DM], reads=[dbgB])
                        K.op("pool", lambda e: e.tensor_copy(out=dbgf, in_=sig[a]), reads=[sigB[a]], writes=[dbgB])
                        K.dma(dbg_sig.rearrange("(n p) d -> p n d", p=128)[:, i, :], dbgf, reads=[dbgB])

            def fin_gen(gn):
                for _ in gn:
                    pass

            g_prev = None
            for i in range(NT):
                gn = d_tile(i)
                next(gn)
                if g_prev is not None:
                    fin_gen(g_prev)
                g_prev = gn
            fin_gen(g_prev)
            A.release(mP)

        x2src = x if stage == 1 else x2d

        K.barrier()
        mE = A.mark()
        w1 = A.alloc([128, 8, DFF], BF16)
        w2 = A.alloc([128, 32, DM], BF16)
        w1B, w2B = Buf("w1"), Buf("w2")
        mS = A.mark()
        stg = [A.alloc([128, 2048], F32) for _ in range(2)]
        stgB = [Buf("stg0"), Buf("stg1")]
        w1v = w1d.rearrange("(c p) f -> p c f", p=128)
        w2v = w2d.rearrange("(c p) n -> p c n", p=128)
        k = 0
        for c in range(8):
            for hf in range(2):
                s_ = k % 2
                K.dma(stg[s_], w1v[:, c, hf * 2048:(hf + 1) * 2048], writes=[stgB[s_]])
                K.op("pool", lambda e, s_=s_, c=c, hf=hf: e.tensor_copy(out=w1[:, c, hf * 2048:(hf + 1) * 2048], in_=stg[s_]),
                     reads=[stgB[s_]], writes=[w1B])
                k += 1
        for fc2 in range(16):
            s_ = k % 2
            K.dma(stg[s_].rearrange("p (a b) -> p a b", b=DM), w2v[:, fc2 * 2:fc2 * 2 + 2, :], writes=[stgB[s_]])
            K.op("pool", lambda e, s_=s_, fc2=fc2: e.tensor_copy(out=w2[:, fc2 * 2:fc2 * 2 + 2, :],
                                                                    in_=stg[s_].rearrange("p (a b) -> p a b", b=DM)),
                 reads=[stgB[s_]], writes=[w2B])
            k += 1

        K.barrier()
        A.release(mS)
        GT = 256
        NG = S // GT
        TPG = GT // 128
        x2t = [A.alloc([128, TPG, DM], F32) for _ in range(2)]
        x2B = [[Buf(f"x2_{a}_{t}") for t in range(TPG)] for a in range(2)]
        h2T = [A.alloc([128, 8, GT], BF16) for _ in range(2)]
        h2B = [Buf("h2T0"), Buf("h2T1")]
        NUR = 6
        uT = A.alloc([128, NUR, GT], BF16)
        uB = [Buf(f"uT{f}") for f in range(NUR)]
        rtmp = [A.alloc([128, GT], F32) for _ in range(2)]
        rtB = [Buf("rt0"), Buf("rt1")]
        wk = {"sq": A.alloc([128, DM], BF16), "ss": A.alloc([128, 1], F32), "rs": A.alloc([128, 1], F32),
              "xn": A.alloc([128, DM], BF16), "B": Buf("nwk"), "bank": 0}
        x3 = [A.alloc([128, DM], F32) for _ in range(2)]
        x3B = [Buf("x3_0"), Buf("x3_1")]
        ss3 = A.alloc([128, 2], F32)
        rs3 = A.alloc([128, 2], F32)
        sB3 = [Buf("s3_0"), Buf("s3_1")]
        ot = [A.alloc([128, DM], F32) for _ in range(2)]
        otB = [Buf("ot0"), Buf("ot1")]
        x2v = x2src.rearrange("(n p) d -> p n d", p=128)
        outv = out.rearrange("(n p) d -> p n d", p=128)
        ubank = [1, 2]
        ybank = [3, 4, 5, 6]
        yi = 0
        ui = 0
        def e_stage1(g):
            a = g % 2
            for t in range(TPG):
                K.dma(x2t[a][:, t, :], x2v[:, g * TPG + t, :], writes=[x2B[a][t]])
                normT_tile(x2t[a][:, t, :], x2B[a][t], gmlp_s, h2T[a][:, :, t * 128:(t + 1) * 128], h2B[a], wk)

        ust = {"ui": 0}
        ybk = [3, 4, 5, 6]

        def e_u(g, f):
            a = g % 2
            bk = ubank[ust["ui"] % 2]
            r_ = ust["ui"] % 2
            ust["ui"] += 1
            for c in range(8):
                K.op("pe", lambda e, bk=bk, c=c: e.matmul(banks[bk][:, 0:GT], w1[:, c, f * 128:(f + 1) * 128],
                                                          h2T[a][:, c, :], start=(c == 0), stop=(c == 7)),
                     reads=[w1B, h2B[a]], writes=[bankB[bk]], signal=(c == 7))
            K.op("act", lambda e, bk=bk, r_=r_: e.activation(out=rtmp[r_], in_=banks[bk][:, 0:GT], func=AF.Relu),
                 reads=[bankB[bk]], writes=[rtB[r_]])
            K.op("dve", lambda e, r_=r_: e.tensor_tensor(out=uT[:, f % NUR, :], in0=rtmp[r_], in1=rtmp[r_], op=ALU.mult),
                 reads=[rtB[r_]], writes=[uB[f % NUR]])

        def e_y(g, f):
            for t in range(TPG):
                for nh in range(2):
                    bk = ybk[t * 2 + nh]
                    K.op("pe", lambda e, bk=bk, t=t, nh=nh: e.matmul(banks[bk][:, :], uT[:, f % NUR, t * 128:(t + 1) * 128],
                                                                     w2[:, f, nh * 512:(nh + 1) * 512],
                                                                     start=(f == 0), stop=(f == 31)),
                         reads=[uB[f % NUR], w2B], writes=[bankB[bk]], signal=(t == TPG - 1 and nh == 1))

        def e_fin(g):
            a = g % 2
            for t in range(TPG):
                p_ = (g * TPG + t) % 2
                for nh in range(2):
                    bk = ybk[t * 2 + nh]
                    K.op("dve", lambda e, bk=bk, p_=p_, nh=nh, t=t: e.tensor_tensor(
                        out=x3[p_][:, nh * 512:(nh + 1) * 512], in0=banks[bk][:, :],
                        in1=x2t[a][:, t, nh * 512:(nh + 1) * 512], op=ALU.add),
                        reads=[bankB[bk], x2B[a][t]], writes=[x3B[p_]])
                K.op("act", lambda e, p_=p_: e.activation(out=wk["sq"], in_=x3[p_], func=AF.Square, accum_out=ss3[:, p_:p_ + 1]),
                     reads=[x3B[p_]], writes=[sB3[p_], wk["B"]])
                rms_scale(ss3[:, p_:p_ + 1], rs3[:, p_:p_ + 1], 1, [sB3[p_]])
                K.op("dve", lambda e, p_=p_: e.scalar_tensor_tensor(out=ot[p_], in0=x3[p_], scalar=rs3[:, p_:p_ + 1], in1=gfin_s,
                                                                     op0=ALU.mult, op1=ALU.mult),
                     reads=[x3B[p_], sB3[p_], cB], writes=[otB[p_]])
                K.dma(outv[:, g * TPG + t, :], ot[p_], reads=[otB[p_]])

        e_stage1(0)
        for g in range(NG):
            e_u(g, 0)
            e_u(g, 1)
            for f in range(32):
                if f + 2 < 32:
                    e_u(g, f + 2)
                e_y(g, f)
                if f == 16 and g + 1 < NG:
                    e_stage1(g + 1)
            e_fin(g)
        A.release(mE)
        stuck, dsig = K.check_deadlock()
        if stuck:
            info = {}
            for e, (p, n) in stuck.items():
                rec = K.ops[e][p]
                info[e] = (p, n, rec[0], [(d.eng, d.idx, d.sem, d.val) for d in rec[2]][:6])
            raise RuntimeError(f"tracker deadlock: {info} done={dsig}")
        K.replay(stack)
    return nc


STAGE = 3
DEBUG = False
_TEST = {}
NITER = 17


def t5_bucket_np(dist):
    n = np.maximum(dist, 0)
    nf = np.maximum(n, 1).astype(np.float32)
    large = 16 + (np.log(nf / np.float32(16)) / np.float32(math.log(128 / 16)) * np.float32(16)).astype(np.int32)
    large = np.minimum(large, 31)
    return np.where(n < 16, n, large)


def fm_block(w, cols):
    blk = w[:, cols]
    return np.ascontiguousarray(blk.reshape(8, 128, 128).transpose(1, 0, 2).reshape(128, 1024))


def host_consts(w, rb):
    f = np.float32
    r = np.arange
    o = {}
    QA, KA, VA, QI, KI, WI, QB, KVB, GB, GBR = 0, 512, 576, 640, 896, 928, 936, 1448, 2216, 2240
    blocks = [r(QA + 128 * b, QA + 128 * (b + 1)) for b in range(4)]
    blocks.append(np.concatenate([r(KA, KA + 64), r(KA, KA + 64)]))
    blocks += [r(QI + 128 * b, QI + 128 * (b + 1)) for b in range(2)]
    blocks.append(np.concatenate([r(KI, KI + 32)] * 4))
    o["wfa"] = np.stack([fm_block(w, c) for c in blocks])
    ta = w[:, np.concatenate([r(VA, VA + 64), r(WI, WI + 8)])]
    o["wta"] = np.ascontiguousarray(ta.reshape(8, 128, 72).transpose(1, 0, 2).reshape(128, 8 * 72))
    o["wg"] = np.ascontiguousarray(w[:, GBR:GBR + 2048])
    sp = r(128)[:, None]
    tp = r(128)[None, :]
    d0 = rb[t5_bucket_np(tp - sp)]
    d1 = rb[t5_bucket_np(128 + tp - sp)]
    dg = np.stack([d0, d1], axis=2)
    o["dgT"] = np.ascontiguousarray(dg.transpose(0, 3, 2, 1).reshape(128, 16 * 2 * 128))
    o["c31"] = np.ascontiguousarray(rb[31:32, :])
    o["cnegT"] = np.where(sp > tp, f(NEG), f(0)).astype(f)
    o["ctok"] = np.where(tp > sp, f(-1e30), f(0)).astype(f)
    o["pow2"] = (2.0 ** (-np.arange(NITER, dtype=np.float64))).astype(f)[None, :]
    if STAGE >= 3:
        KC, VC, KS, VS, KW, VW = (KVB + 128 * k for k in range(6))
        blocks = [r(QB + 128 * b, QB + 128 * (b + 1)) for b in range(4)]
        for base in (KS, KW):
            for g in range(2):
                blocks.append(np.concatenate([r(base + 64 * g, base + 64 * g + 64)] * 2))
        blocks.append(r(KC, KC + 128))
        blocks.append(r(VC, VC + 128))
        o["wfb"] = np.stack([fm_block(w, c) for c in blocks])
        tb = w[:, np.concatenate([r(VS, VS + 128), r(VW, VW + 128), r(GB, GB + 24)])]
        o["wtb"] = np.ascontiguousarray(tb.reshape(8, 128, 280).transpose(1, 0, 2).reshape(128, 8 * 280))
        cc = r(503)[None, :]
        dist = sp - 16 * (cc - 248) - 31
        bcg = rb[t5_bucket_np(dist)][:, :, 8:16]
        o["bcg"] = np.ascontiguousarray(bcg.transpose(0, 2, 1).reshape(128, 8 * 503))
        o["bcm"] = np.where(dist >= 0, f(0), f(NEG)).astype(f)
        c = r(255)[:, None]
        n = r(64)[None, :]
        ovl = np.clip(np.minimum(16 * c + 32, 64 * n + 64) - np.maximum(16 * c, 64 * n), 0, None).astype(f) / f(32)
        ovp = np.zeros((256, 64), f)
        ovp[:255] = ovl
        o["ov"] = np.ascontiguousarray(ovp.reshape(2, 128, 64).transpose(1, 0, 2).reshape(128, 128))
        rel = r(126)[None, :] - 62
        cl = (r(128) // 64)[:, None]
        forced_cur = rel == cl
        forced_prev = rel == cl - 1
        invalid = rel > cl
        o["selm0"] = np.where(forced_cur | forced_prev | invalid, f(0), f(1)).astype(f)
        o["selm1"] = np.where(invalid, f(-1e30), np.where(forced_cur, f(20000), np.where(forced_prev, f(10000), f(0)))).astype(f)
        o["wfar"] = np.where(sp > tp, f(0), f(NEG)).astype(f)
        o["eexp"] = (r(64)[:, None] == (r(S)[None, :] // 64)).astype(f)
    return o


def cmp_consts(w1k, w2k, pek, w1v, w2v, pev):
    o = {}
    for nm, w1, pe in (("k", w1k, pek), ("v", w1v, pev)):
        w1r = w1.reshape(32, 64, 128).transpose(1, 0, 2)
        o["w1" + nm] = np.ascontiguousarray(np.concatenate([w1r, w1r], axis=0).reshape(128, 32 * 128))
        pt = np.ascontiguousarray(pe.T)
        o["pe" + nm] = np.ascontiguousarray(np.concatenate([pt, pt], axis=0))
    o["w2k"] = np.ascontiguousarray(np.concatenate([w2k, w2k], axis=1))
    o["w2v"] = np.ascontiguousarray(w2v)
    return o


def kernel(x, norm_mix, w_in, cmp_pe_k, cmp_w1_k, cmp_w2_k, cmp_pe_v, cmp_w1_v, cmp_w2_v,
           rel_bias, w_branch_a, w_branch_b, w_out, norm_mlp, w_mlp_in, w_mlp_out, norm_final):
    f = np.float32
    x = np.asarray(x, f)
    B = x.shape[0]
    nc = build_program(STAGE)
    common = {
        "gmix": np.ascontiguousarray(np.asarray(norm_mix, f)[0].reshape(8, 128).T),
        "gmlp": np.ascontiguousarray(np.asarray(norm_mlp, f)[0].reshape(8, 128).T),
        "gfin": np.ascontiguousarray(np.asarray(norm_final, f).reshape(1, DM)),
        "w_mlp_in": np.ascontiguousarray(np.asarray(w_mlp_in, f)[0]),
        "w_mlp_out": np.ascontiguousarray(np.asarray(w_mlp_out, f)[0]),
        "ident": np.eye(128, dtype=f),
    }
    if STAGE >= 2:
        common.update(host_consts(np.asarray(w_in, f)[0], np.asarray(rel_bias, f)))
        common["wba"] = np.ascontiguousarray(np.asarray(w_branch_a, f)[0])
        common["wbb"] = np.ascontiguousarray(np.asarray(w_branch_b, f)[0])
        common["wo"] = np.ascontiguousarray(np.asarray(w_out, f)[0])
    if STAGE >= 3:
        common.update(cmp_consts(*(np.asarray(a, f)[0] for a in (cmp_w1_k, cmp_w2_k, cmp_pe_k, cmp_w1_v, cmp_w2_v, cmp_pe_v))))
    if STAGE == 4:
        common.update(_TEST)
    in_maps = []
    for b in range(B):
        m = dict(common)
        m["x"] = np.ascontiguousarray(x[b])
        in_maps.append(m)
    res = run_bass_kernel_spmd(nc, in_maps, core_ids=list(range(B)))
    global _LAST
    _LAST = res
    return np.stack([np.asarray(r["out"], f) for r in res.results], axis=0)
```

```python
import math
from contextlib import ExitStack

import numpy as np
import concourse.bass as bass
import concourse.mybir as mybir
from concourse.bass_utils import run_bass_kernel_spmd

F32 = mybir.dt.float32
BF16 = mybir.dt.bfloat16
U8 = mybir.dt.uint8
ALU = mybir.AluOpType
AF = mybir.ActivationFunctionType
AX = mybir.AxisListType

S = 4096
DM = 1024
NT = S // 128
DFF = 4096
EPS = 1e-6
NEG = -30000.0
EPOCH = 6000
NDMASEM = 24
INORDER = ("pe", "act", "dve", "pool")


class Ev:
    __slots__ = ("eng", "idx", "sem", "val")

    def __init__(self, eng, idx=None, sem=None, val=None):
        self.eng, self.idx, self.sem, self.val = eng, idx, sem, val


class Buf:
    __slots__ = ("name", "w", "r", "rd")

    def __init__(self, name):
        self.name, self.w, self.r, self.rd = name, None, {}, []


class KB:
    def __init__(self, nc):
        self.nc = nc
        self.ops = {e: [] for e in INORDER + ("sp",)}
        self.nsig = {e: 0 for e in INORDER}
        self.dma_next = 0
        self.dma_val = [0] * NDMASEM
        self.dma_out = []
        self.all_dma = []

    def _deps(self, reads, writes):
        deps = []
        for b in reads:
            if b.w is not None:
                deps.append(b.w)
        for b in writes:
            if b.w is not None:
                deps.append(b.w)
            deps.extend(b.r.values())
            deps.extend(b.rd)
        return deps

    def _mark(self, ev, reads, writes):
        for b in reads:
            if ev.eng == "dma":
                b.rd.append(ev)
            else:
                b.r[ev.eng] = ev
        for b in writes:
            b.w = ev
            b.r = {}
            b.rd = []

    def op(self, eng, emit, reads=(), writes=(), signal=True, extra=()):
        deps = self._deps(reads, writes) + list(extra)
        deps = [d for d in deps if not (d.eng == eng and d.idx >= self.nsig[eng])]
        ev = Ev(eng, self.nsig[eng])
        if signal:
            self.nsig[eng] += 1
        self.ops[eng].append(("op", emit, deps, ev if signal else None))
        self._mark(ev, reads, writes)
        return ev

    def dma(self, out, in_, reads=(), writes=(), q="sp", extra=()):
        deps = self._deps(reads, writes) + list(extra)
        k = self.dma_next
        self.dma_next = (k + 1) % NDMASEM
        prev = self.dma_val[k]
        self.dma_val[k] = prev + 16
        ev = Ev("dma", None, k, prev + 16)
        self.ops[q].append(("dma", (out, in_), deps, ev, prev))
        self._mark(ev, reads, writes)
        self.dma_out.append(ev)
        self.all_dma.append(ev)
        return ev

    def barrier(self):
        evs = [Ev(e, self.nsig[e] - 1) for e in INORDER if self.nsig[e] > 0]
        evs += self.dma_out
        self.dma_out = []
        for e in INORDER + ("sp",):
            self.ops[e].append(("wait", None, list(evs), None))

    def check_deadlock(self):
        engs = list(self.ops.keys())
        ptr = {e: 0 for e in engs}
        done_sig = {e: 0 for e in INORDER}
        done_dma = set()
        progress = True
        while progress:
            progress = False
            for e in engs:
                while ptr[e] < len(self.ops[e]):
                    rec = self.ops[e][ptr[e]]
                    deps = rec[2]
                    ok = True
                    for d in deps:
                        if d.eng == "dma":
                            if id(d) not in done_dma:
                                ok = False
                                break
                        elif d.idx >= done_sig[d.eng]:
                            ok = False
                            break
                    if not ok:
                        break
                    if rec[0] == "op" and rec[3] is not None:
                        done_sig[e] += 1
                    elif rec[0] == "dma":
                        done_dma.add(id(rec[3]))
                    ptr[e] += 1
                    progress = True
        stuck = {e: (ptr[e], len(self.ops[e])) for e in engs if ptr[e] < len(self.ops[e])}
        return stuck, done_sig

    def replay(self, stack):
        nc = self.nc
        sems = {}
        for e in INORDER:
            n = self.nsig[e] // EPOCH + 1
            sems[e] = [stack.enter_context(nc.semaphore(f"s_{e}{i}")) for i in range(n)]
        dsem = [stack.enter_context(nc.semaphore(f"s_d{i}")) for i in range(NDMASEM)]
        final_evs = [Ev(e, self.nsig[e] - 1) for e in INORDER if self.nsig[e] > 0] + self.all_dma[-64:]
        self.ops["sp"].append(("wait", None, final_evs, None))
        block = stack.enter_context(nc.Block())

        def run(eng_name):
            def body(e):
                waited = {a: -1 for a in INORDER}
                wd = [0] * NDMASEM

                def do_waits(deps):
                    need = {}
                    for d in deps:
                        if d.eng == "dma":
                            if d.val > wd[d.sem]:
                                wd[d.sem] = d.val
                                e.wait_ge(dsem[d.sem], d.val)
                        else:
                            if d.idx > waited[d.eng]:
                                need[d.eng] = max(need.get(d.eng, -1), d.idx)
                    for a, idx in need.items():
                        waited[a] = idx
                        e.wait_ge(sems[a][idx // EPOCH], idx % EPOCH + 1)

                for rec in self.ops[eng_name]:
                    kind = rec[0]
                    if kind == "wait":
                        do_waits(rec[2])
                    elif kind == "op":
                        _, emit, deps, ev = rec
                        do_waits(deps)
                        ins = emit(e)
                        if ev is not None:
                            ins.then_inc(sems[ev.eng][ev.idx // EPOCH], 1)
                    else:
                        _, (out, in_), deps, ev, prev = rec
                        do_waits(deps)
                        if prev > wd[ev.sem]:
                            wd[ev.sem] = prev
                            e.wait_ge(dsem[ev.sem], prev)
                        e.dma_start(out=out, in_=in_).then_inc(dsem[ev.sem], 16)
            return body

        block.tensor(run("pe"))
        block.scalar(run("act"))
        block.vector(run("dve"))
        block.gpsimd(run("pool"))
        block.sync(run("sp"))


class Arena:
    def __init__(self, ap, nwords):
        self.ap, self.n, self.top = ap, nwords, 0

    def alloc(self, shape, dt, name=None):
        free = int(np.prod(shape[1:]))
        bpe = {F32: 4, BF16: 2, U8: 1}[dt]
        words = (free * bpe + 3) // 4
        off = self.top
        self.top += words
        assert self.top <= self.n, f"SBUF arena overflow {self.top} > {self.n} ({name})"
        v = self.ap[:, off:off + words]
        if dt != F32:
            v = v.bitcast(dt)
        v = v[:, 0:free]
        if len(shape) == 3:
            v = v.rearrange("p (a b) -> p a b", b=shape[2])
        elif len(shape) == 4:
            v = v.rearrange("p (a b c) -> p a b c", b=shape[2], c=shape[3])
        return v

    def mark(self):
        return self.top

    def release(self, m):
        self.top = m


def build_program(stage):
    nc = bass.Bass("TRN2", target_bir_lowering=False)
    D = {}

    def din(name, shape, dt=F32):
        D[name] = nc.dram_tensor(name, list(shape), dt, kind="ExternalInput").ap()
        return D[name]

    x = din("x", [S, DM])
    gmix = din("gmix", [128, 8])
    gmlp = din("gmlp", [128, 8])
    gfin = din("gfin", [1, DM])
    w1d = din("w_mlp_in", [DM, DFF])
    w2d = din("w_mlp_out", [DFF, DM])
    identd = din("ident", [128, 128])
    out = nc.dram_tensor("out", [S, DM], F32, kind="ExternalOutput").ap()
    x2d = nc.dram_tensor("x2_scratch", [S, DM], F32, kind="Internal").ap()

    stack = ExitStack()
    with stack:
        NW = 53150
        arena_t = stack.enter_context(nc.sbuf_tensor("arena", [128, NW], F32))
        A = Arena(arena_t, NW)
        banks = [stack.enter_context(nc.psum_tensor(f"bank{i}", [128, 512], F32)) for i in range(8)]
        bankB = [Buf(f"bank{i}") for i in range(8)]
        K = KB(nc)

        ident_f = A.alloc([128, 128], F32)
        ident = A.alloc([128, 128], BF16)
        gmix_s = A.alloc([128, 8], F32)
        gmlp_s = A.alloc([128, 8], F32)
        gfin_s = A.alloc([128, DM], F32)
        cB = Buf("consts")
        K.dma(ident_f, identd, writes=[cB])
        K.dma(gmix_s, gmix, writes=[cB])
        K.dma(gmlp_s, gmlp, writes=[cB])
        K.dma(gfin_s, gfin.partition_broadcast(128), writes=[cB])
        K.op("dve", lambda e: e.tensor_copy(out=ident, in_=ident_f), reads=[cB], writes=[cB])

        def rms_scale(ss, rs, n, bufs):
            K.op("dve", lambda e: e.tensor_scalar(out=rs, in0=ss, scalar1=1.0 / DM, scalar2=EPS,
                                                  op0=ALU.mult, op1=ALU.add), reads=bufs, writes=bufs)
            K.op("act", lambda e: e.activation(out=rs, in_=rs, func=AF.Sqrt), reads=bufs, writes=bufs)
            K.op("dve", lambda e: e.reciprocal(out=rs, in_=rs), reads=bufs, writes=bufs)

        def normT_tile(xt, xtB, gT, dst3, dstB, wk):
            sq, ss, rs, xn, sB = wk["sq"], wk["ss"], wk["rs"], wk["xn"], wk["B"]
            K.op("act", lambda e: e.activation(out=sq, in_=xt, func=AF.Square, accum_out=ss),
                 reads=[xtB], writes=[sB])
            rms_scale(ss, rs, 1, [sB])
            K.op("dve", lambda e: e.tensor_scalar(out=xn, in0=xt, scalar1=rs, scalar2=None, op0=ALU.mult),
                 reads=[xtB, sB], writes=[sB])
            bk = wk["bank"]
            pv = banks[bk][:, 0:512].bitcast(BF16)
            for c in range(8):
                K.op("pe", lambda e, c=c: e.transpose(pv[:, c * 128:(c + 1) * 128], xn[:, c * 128:(c + 1) * 128], ident),
                     reads=[sB, cB], writes=[bankB[bk]], signal=(c == 7))
            K.op("dve", lambda e: e.tensor_tensor(out=dst3, in0=pv.rearrange("p (a b) -> p a b", b=128),
                                                  in1=gT.unsqueeze(2).to_broadcast([128, 8, 128]), op=ALU.mult),
                 reads=[bankB[bk], cB], writes=[dstB])


        NIT = 17
        xv = x.rearrange("(n p) d -> p n d", p=128)
        x2v_w = x2d.rearrange("(n p) d -> p n d", p=128)
        if stage >= 2:
            wfa = din("wfa", [8, 128, 1024])
            wta = din("wta", [128, 8 * 72])
            dgT = din("dgT", [128, 16 * 2 * 128])
            c31d = din("c31", [1, 16])
            cnegTd = din("cnegT", [128, 128])
            ctokd = din("ctok", [128, 128])
            p2d = din("pow2", [1, NIT])
            wbad = din("wba", [512, DM])
            wbbd = din("wbb", [512, DM])
            wod = din("wo", [DM, DM])
            wgd = din("wg", [DM, 2048])
            if DEBUG:
                dbg_oa = nc.dram_tensor("dbg_oa", [S, 512], BF16, kind="ExternalOutput").ap()
                dbg_ob = nc.dram_tensor("dbg_ob", [S, 512], F32, kind="ExternalOutput").ap()
                dbg_x2 = nc.dram_tensor("dbg_x2", [S, DM], F32, kind="ExternalOutput").ap()
                dbg_mix = nc.dram_tensor("dbg_mix", [S, DM], F32, kind="ExternalOutput").ap()
                dbg_sig = nc.dram_tensor("dbg_sig", [S, 2048], F32, kind="ExternalOutput").ap()
                dbg_ya = nc.dram_tensor("dbg_ya", [S, DM], F32, kind="ExternalOutput").ap()
                dbg_hT = nc.dram_tensor("dbg_hT", [128, 1024], F32, kind="ExternalOutput").ap()
                dbg_z = nc.dram_tensor("dbg_z", [128, 512], F32, kind="ExternalOutput").ap()
                dbg_wg = nc.dram_tensor("dbg_wg", [128, 2048], F32, kind="ExternalOutput").ap()

            c31 = A.alloc([128, 16], F32)
            zeros_sb = A.alloc([128, 260], F32)
            K.op("pool", lambda e: e.memset(zeros_sb, 0.0), writes=[cB])
            ctok = A.alloc([128, 128], F32)
            pow2 = A.alloc([128, NIT], F32)
            Dp = A.alloc([128, 16, 2, 128], BF16)
            K.dma(c31, c31d.partition_broadcast(128), writes=[cB])
            K.dma(ctok, ctokd, writes=[cB])
            K.dma(pow2, p2d.partition_broadcast(128), writes=[cB])
            mP = A.mark()
            if stage == 4:
                oa_scr = din("t_oa", [S, 512], BF16).rearrange("(n p) d -> p n d", p=128)
                ob_scr = din("t_ob", [S, 512], BF16).rearrange("(n p) d -> p n d", p=128)
            else:
                oa_scr = nc.dram_tensor("oa_scr", [S, 512], BF16, kind="Internal").ap().rearrange("(n p) d -> p n d", p=128)
                ob_scr = nc.dram_tensor("ob_scr", [S, 512], BF16, kind="Internal").ap().rearrange("(n p) d -> p n d", p=128)
            mB0 = A.mark()
            dgs = A.alloc([128, 16, 2, 128], F32)
            cnegT = A.alloc([128, 128], F32)
            K.dma(dgs, dgT.rearrange("p (a b c) -> p a b c", b=2, c=128), writes=[cB])
            K.dma(cnegT, cnegTd, writes=[cB])
            for hh in range(16):
                K.op("dve", lambda e, hh=hh: e.scalar_tensor_tensor(out=Dp[:, hh, 0, :], in0=dgs[:, hh, 0, :], scalar=c31[:, hh:hh + 1],
                                                                     in1=cnegT, op0=ALU.subtract, op1=ALU.add),
                     reads=[cB], writes=[cB])
                K.op("dve", lambda e, hh=hh: e.tensor_scalar(out=Dp[:, hh, 1, :], in0=dgs[:, hh, 1, :], scalar1=c31[:, hh:hh + 1],
                                                              scalar2=None, op0=ALU.subtract),
                     reads=[cB], writes=[cB])
            K.barrier()
            A.release(mB0)

            def compute_hT(hT, hTB, wk_, nbuf=2):
                xt = [A.alloc([128, DM], F32) for _ in range(nbuf)]
                xtB = [Buf(f"xt{q}") for q in range(nbuf)]
                for i in range(NT):
                    K.dma(xt[i % nbuf], xv[:, i, :], writes=[xtB[i % nbuf]])
                    normT_tile(xt[i % nbuf], xtB[i % nbuf], gmix_s, hT[:, :, i * 128:(i + 1) * 128], hTB, wk_)

            pj = {"k": 0}

            def proj_fm(wsrc, hT, hTB, dst, dstB, wst, wstB, wb, wbB, scale, pbanks):
                K.dma(wst, wsrc, writes=[wstB])
                K.op("pool", lambda e: e.tensor_copy(out=wb, in_=wst), reads=[wstB], writes=[wbB])
                for tg in range(8):
                    bk = pbanks[pj["k"] % len(pbanks)]
                    pj["k"] += 1
                    for c in range(8):
                        K.op("pe", lambda e, bk=bk, c=c, tg=tg: e.matmul(banks[bk][:, :], wb[:, c * 128:(c + 1) * 128],
                                                                         hT[:, c, tg * 512:(tg + 1) * 512], start=(c == 0), stop=(c == 7)),
                             reads=[wbB, hTB], writes=[bankB[bk]], signal=(c == 7))
                    K.op("act", lambda e, bk=bk, tg=tg: e.mul(out=dst[:, tg * 512:(tg + 1) * 512], in_=banks[bk][:, :], mul=float(scale)),
                         reads=[bankB[bk]], writes=[dstB])

            def attn_phases():
                mB = A.mark()
                qaT = A.alloc([128, 4, S], BF16)
                kaT2 = A.alloc([128, S], BF16)
                qiT = A.alloc([128, 2, S], BF16)
                kiT4 = A.alloc([128, S], BF16)
                vaug = A.alloc([128, NT, 65], BF16)
                wabs = A.alloc([128, NT, 8], F32)
                wsgn = A.alloc([128, NT, 8], F32)
                qaB, kaB, qiB, kiB, vaB, wiB = Buf("qaT"), Buf("kaT2"), Buf("qiT"), Buf("kiT4"), Buf("vaug"), Buf("wi")
                mB1 = A.mark()
                hT = A.alloc([128, 8, S], BF16)
                hTB = Buf("hT")
                wk1 = {"sq": A.alloc([128, DM], BF16), "ss": A.alloc([128, 1], F32), "rs": A.alloc([128, 1], F32),
                       "xn": A.alloc([128, DM], BF16), "B": Buf("nwk1"), "bank": 0}
                compute_hT(hT, hTB, wk1)
                wst = A.alloc([128, 1024], F32)
                wb = [A.alloc([128, 1024], BF16) for _ in range(2)]
                wstB, wbB = Buf("wst"), [Buf("wb0"), Buf("wb1")]
                dsts = [(qaT[:, 0, :], qaB, 0.125), (qaT[:, 1, :], qaB, 0.125), (qaT[:, 2, :], qaB, 0.125), (qaT[:, 3, :], qaB, 0.125),
                        (kaT2, kaB, 1.0), (qiT[:, 0, :], qiB, 1.0), (qiT[:, 1, :], qiB, 1.0), (kiT4, kiB, 1.0)]
                for b_, (dst, dB, sc) in enumerate(dsts):
                    proj_fm(wfa[b_], hT, hTB, dst, dB, wst, wstB, wb[b_ % 2], wbB[b_ % 2], sc, [1, 2])
                wtf = A.alloc([128, 8 * 72], F32)
                wtb_ = A.alloc([128, 8, 72], BF16)
                wtB = Buf("wta")
                K.dma(wtf, wta, writes=[wtB])
                K.op("pool", lambda e: e.tensor_copy(out=wtb_, in_=wtf.rearrange("p (a b) -> p a b", b=72)), reads=[wtB], writes=[wtB])
                K.op("pool", lambda e: e.memset(vaug[:, :, 64:65], 1.0), writes=[vaB])
                for i in range(NT):
                    bk = [3, 4][i % 2]
                    for c in range(8):
                        K.op("pe", lambda e, bk=bk, c=c, i=i: e.matmul(banks[bk][:, 0:72], hT[:, c, i * 128:(i + 1) * 128], wtb_[:, c, :],
                                                                       start=(c == 0), stop=(c == 7)),
                             reads=[hTB, wtB], writes=[bankB[bk]], signal=(c == 7))
                    K.op("act", lambda e, bk=bk, i=i: e.activation(out=vaug[:, i, 0:64], in_=banks[bk][:, 0:64], func=AF.Copy),
                         reads=[bankB[bk]], writes=[vaB])
                    K.op("act", lambda e, bk=bk, i=i: e.activation(out=wabs[:, i, :], in_=banks[bk][:, 64:72], func=AF.Abs),
                         reads=[bankB[bk]], writes=[wiB])
                    K.op("act", lambda e, bk=bk, i=i: e.activation(out=wsgn[:, i, :], in_=banks[bk][:, 64:72], func=AF.Sign),
                         reads=[bankB[bk]], writes=[wiB])
                K.barrier()
                A.release(mB1)

                score2 = [A.alloc([128, S], F32) for _ in range(2)]
                scB2 = [Buf("score0"), Buf("score1")]
                mneg = [A.alloc([128, S], BF16) for _ in range(2)]
                mnB = [Buf("mneg0"), Buf("mneg1")]
                mnegT2 = [A.alloc([128, NT, 512], BF16) for _ in range(2)]
                mtB2 = [Buf("mnegT0"), Buf("mnegT1")]
                rbuf = [A.alloc([128, 512], BF16) for _ in range(2)]
                rbB = [Buf("rb0"), Buf("rb1")]
                pT = [A.alloc([128, 512], BF16) for _ in range(3)]
                pTB = [Buf(f"pT{i}") for i in range(3)]
                bs2 = [A.alloc([128, 16], F32) for _ in range(2)]
                stepv2 = [A.alloc([128, NIT], F32) for _ in range(2)]
                bsB2 = [Buf("bs0"), Buf("bs1")]
                rinv = A.alloc([128, 4], F32)
                rvB = Buf("rinv")
                oa_st = [A.alloc([128, 4, 512], BF16)] * 2
                oaB = [Buf("oast0")] * 2
                if DEBUG:
                    dbv = dbg_oa.rearrange("(n p) d -> p n d", p=128)
                AT = {"pT": pT, "pTB": pTB, "rinv": rinv, "rvB": rvB, "abanks": [5, 6, 0], "sbanks": [3, 4]}
                WSCALE = (8 ** -0.5) * (32 ** -0.5)
                cnt = {"ix": 0, "r": 0, "sc": 0, "p": 0, "acc": 0, "tr": 0}

                BG = {"gens": [], "rate": 1, "acc": 0.0}

                def bg_one():
                    w = BG.get("width", 1)
                    k = BG.get("rr", 0) % min(w, len(BG["gens"]))
                    BG["rr"] = BG.get("rr", 0) + 1
                    try:
                        next(BG["gens"][k])
                    except StopIteration:
                        BG["gens"].pop(k)

                def bg_step():
                    BG["acc"] += BG["rate"]
                    while BG["acc"] >= 1.0 and BG["gens"]:
                        BG["acc"] -= 1.0
                        bg_one()
                    if not BG["gens"]:
                        BG["acc"] = 0.0

                def bg_drain():
                    while BG["gens"]:
                        bg_one()
                    BG["acc"] = 0.0

                def dsa_indexer(i):
                    q2 = i % 2
                    score, scB, bs, bsB, stepv = score2[q2], scB2[q2], bs2[q2], bsB2[q2], stepv2[q2]
                    Wd = 128 * (i + 1)
                    nch = (Wd + 511) // 512
                    for ch in range(nch):
                        cols = min(512, Wd - 512 * ch)
                        for h in range(8):
                            bk = [1, 2][cnt["ix"] % 2]
                            cnt["ix"] += 1
                            pb = 32 * (h % 4)
                            K.op("pe", lambda e, bk=bk, h=h, pb=pb, ch=ch, cols=cols: e.matmul(
                                banks[bk][:, 0:cols], qiT[pb:pb + 32, h // 4, i * 128:(i + 1) * 128],
                                kiT4[pb:pb + 32, ch * 512:ch * 512 + cols], start=True, stop=True, tile_position=(pb, 0)),
                                reads=[qiB, kiB], writes=[bankB[bk]])
                            r_ = cnt["r"] % 2
                            cnt["r"] += 1
                            K.op("act", lambda e, bk=bk, r_=r_, h=h, cols=cols: e.activation(
                                out=rbuf[r_][:, 0:cols], in_=banks[bk][:, 0:cols], func=AF.Relu, scale=wabs[:, i, h:h + 1]),
                                reads=[bankB[bk], wiB], writes=[rbB[r_]])
                            sl = score[:, ch * 512:ch * 512 + cols]
                            if h == 0:
                                K.op("dve", lambda e, r_=r_, h=h, cols=cols, sl=sl: e.tensor_scalar(
                                    out=sl, in0=rbuf[r_][:, 0:cols], scalar1=wsgn[:, i, h:h + 1], scalar2=None, op0=ALU.mult),
                                    reads=[rbB[r_], wiB], writes=[scB])
                            else:
                                K.op("dve", lambda e, r_=r_, h=h, cols=cols, sl=sl: e.scalar_tensor_tensor(
                                    out=sl, in0=rbuf[r_][:, 0:cols], scalar=wsgn[:, i, h:h + 1], in1=sl, op0=ALU.mult, op1=ALU.add),
                                    reads=[rbB[r_], wiB, scB], writes=[scB])
                            yield
                    sw = score[:, 0:Wd]
                    K.op("dve", lambda e: e.tensor_reduce(out=bs[:, 0:1], in_=sw, axis=AX.X, op=ALU.min), reads=[scB], writes=[bsB])
                    K.op("dve", lambda e: e.tensor_tensor(out=score[:, i * 128:(i + 1) * 128], in0=score[:, i * 128:(i + 1) * 128],
                                                          in1=ctok, op=ALU.add), reads=[scB, cB], writes=[scB])
                    K.op("dve", lambda e: e.tensor_reduce(out=bs[:, 1:2], in_=sw, axis=AX.X, op=ALU.max), reads=[scB], writes=[bsB])
                    K.op("dve", lambda e: e.tensor_tensor(out=bs[:, 2:3], in0=bs[:, 1:2], in1=bs[:, 0:1], op=ALU.subtract),
                         reads=[bsB], writes=[bsB])
                    K.op("dve", lambda e: e.tensor_scalar(out=bs[:, 2:3], in0=bs[:, 2:3], scalar1=1.0, scalar2=0.5, op0=ALU.add, op1=ALU.mult),
                         reads=[bsB], writes=[bsB])
                    K.op("dve", lambda e: e.scalar_tensor_tensor(out=bs[:, 3:4], in0=bs[:, 0:1], scalar=-1.0, in1=bs[:, 2:3],
                                                                 op0=ALU.add, op1=ALU.add), reads=[bsB], writes=[bsB])
                    K.op("dve", lambda e: e.tensor_scalar(out=stepv, in0=pow2, scalar1=bs[:, 2:3], scalar2=None, op0=ALU.mult),
                         reads=[bsB, cB], writes=[bsB])
                    yield
                    for k in range(NIT):
                        K.op("dve", lambda e: e.tensor_scalar(out=mneg[q2][:, 0:Wd], in0=sw, scalar1=bs[:, 3:4], scalar2=0.0,
                                                              op0=ALU.is_gt, op1=ALU.add, accum_out=bs[:, 4:5]),
                             reads=[scB, bsB], writes=[mnB[q2], bsB])
                        K.op("dve", lambda e: e.tensor_scalar(out=bs[:, 5:6], in0=bs[:, 4:5], scalar1=256.0, scalar2=-0.5,
                                                              op0=ALU.is_ge, op1=ALU.add), reads=[bsB], writes=[bsB])
                        K.op("dve", lambda e, k=k: e.scalar_tensor_tensor(out=bs[:, 3:4], in0=bs[:, 5:6], scalar=stepv[:, k:k + 1],
                                                                          in1=bs[:, 3:4], op0=ALU.mult, op1=ALU.add),
                             reads=[bsB], writes=[bsB])
                        yield
                    m_ = i % 2
                    K.op("dve", lambda e: e.tensor_tensor(out=bs[:, 3:4], in0=bs[:, 3:4], in1=stepv[:, NIT - 1:NIT], op=ALU.subtract),
                         reads=[bsB], writes=[bsB])
                    K.op("dve", lambda e, m_=m_: e.tensor_scalar(out=mneg[m_][:, 0:Wd], in0=sw, scalar1=bs[:, 3:4], scalar2=NEG,
                                                                 op0=ALU.is_le, op1=ALU.mult), reads=[scB, bsB], writes=[mnB[m_]])
                    tl = i % 4
                    j0 = 0
                    while j0 <= i:
                        nb = min(8, i + 1 - j0)
                        bk = 7
                        pv = banks[bk][:, 0:512].bitcast(BF16)
                        for jj in range(nb):
                            j = j0 + jj
                            K.op("pe", lambda e, j=j, jj=jj, m_=m_: e.transpose(pv[:, jj * 128:(jj + 1) * 128],
                                                                                 mneg[m_][:, j * 128:(j + 1) * 128], ident),
                                 reads=[mnB[m_], cB], writes=[bankB[bk]], signal=(jj == nb - 1))
                        K.op("act", lambda e, j0=j0, nb=nb, tl=tl: e.activation(
                            out=mnegT2[(i // 4) % 2][:, j0:j0 + nb, tl * 128:(tl + 1) * 128],
                            in_=pv[:, 0:nb * 128].rearrange("p (a b) -> p a b", b=128), func=AF.Copy),
                            reads=[bankB[bk]], writes=[mtB2[(i // 4) % 2]])
                        j0 += nb
                        yield

                def attention(T, h, hh, qT, qB_, kT, kB_, vA, vB_, jlist, maskT, maskB, kinds, writer, span=1000):
                    pb = 64 * (h % 2)
                    pT, pTB = AT["pT"], AT["pTB"]
                    npT = len(pT)
                    abanks = AT["abanks"]
                    sbanks = AT["sbanks"]
                    ab = abanks[cnt["acc"] % len(abanks)]
                    cnt["acc"] += 1
                    K.op("act", lambda e, ab=ab: e.activation(out=banks[ab][:, 0:260], in_=zeros_sb[:, 0:260], func=AF.Copy),
                         reads=[cB], writes=[bankB[ab]])

                    def stageA(j):
                        i_min = max(j, 4 * T)
                        i_max = min(4 * T + 3, j + span)
                        col0 = (i_min - 4 * T) * 128
                        col1 = (i_max - 4 * T + 1) * 128
                        bk = sbanks[cnt["sc"] % len(sbanks)]
                        cnt["sc"] += 1
                        extra = []
                        mt = maskT(j)
                        if mt is not None:
                            extra.append(("m", mt))
                        for i in range(i_min, i_max + 1):
                            kd = kinds(j, i)
                            if kd is not None:
                                extra.append(("b", i, kd))
                        K.op("pe", lambda e, bk=bk, j=j, col0=col0, col1=col1, ne=len(extra): e.matmul(
                            banks[bk][:, col0:col1], kT[pb:pb + 64, j * 128:(j + 1) * 128],
                            qT[pb:pb + 64, T * 512 + col0:T * 512 + col1], start=True, stop=(ne == 0)),
                            reads=[kB_, qB_], writes=[bankB[bk]], signal=(len(extra) == 0))
                        for n_, ex in enumerate(extra):
                            last = (n_ == len(extra) - 1)
                            if ex[0] == "m":
                                ml, mr = ex[1] if isinstance(ex[1], tuple) else (ident, ex[1])
                                K.op("pe", lambda e, bk=bk, col0=col0, col1=col1, ml=ml, mr=mr, last=last: e.matmul(
                                    banks[bk][:, col0:col1], ml, mr[:, col0:col1], start=False, stop=last),
                                    reads=[maskB, cB], writes=[bankB[bk]], signal=last)
                            else:
                                _, i, kd = ex
                                c0 = (i - 4 * T) * 128
                                K.op("pe", lambda e, bk=bk, c0=c0, kd=kd, last=last: e.matmul(
                                    banks[bk][:, c0:c0 + 128], ident, kd, start=False, stop=last),
                                    reads=[cB], writes=[bankB[bk]], signal=last)
                        return (j, bk, col0, col1, i_min, i_max)

                    def stageB(info):
                        j, bk, col0, col1, i_min, i_max = info
                        p_ = cnt["p"] % npT
                        cnt["p"] += 1
                        K.op("act", lambda e, bk=bk, p_=p_, col0=col0, col1=col1: e.activation(
                            out=pT[p_][:, col0:col1], in_=banks[bk][:, col0:col1], func=AF.Exp, bias=c31[:, hh:hh + 1]),
                            reads=[bankB[bk], cB], writes=[pTB[p_]])
                        return p_

                    def stageC(info, p_):
                        j, bk, col0, col1, i_min, i_max = info
                        ntl = i_max + 1 - i_min
                        for n_, i in enumerate(range(i_min, i_max + 1)):
                            tl = i - 4 * T
                            K.op("pe", lambda e, ab=ab, p_=p_, tl=tl, j=j: e.matmul(
                                banks[ab][:, tl * 65:(tl + 1) * 65], pT[p_][:, tl * 128:(tl + 1) * 128], vA[:, j, :],
                                start=False, stop=False, skip_group_check=True),
                                reads=[pTB[p_], vB_], writes=[bankB[ab]], signal=(n_ == ntl - 1))

                    prev = None
                    for j in jlist:
                        info = stageA(j)
                        if prev is not None:
                            stageC(*prev)
                        p_ = stageB(info)
                        prev = (info, p_)
                        bg_step()
                    stageC(*prev)
                    writer(ab)

                def dsa_head(T, h):
                    def writer(ab):
                        accv = banks[ab][:, 0:260].rearrange("p (a b) -> p a b", b=65)
                        K.op("dve", lambda e: e.reciprocal(out=rinv, in_=accv[:, :, 64]), reads=[bankB[ab]], writes=[rvB])
                        for tl in range(4):
                            K.op("dve", lambda e, tl=tl: e.tensor_scalar(out=oa_st[T % 2][:, tl, h * 64:(h + 1) * 64], in0=accv[:, tl, 0:64],
                                                                          scalar1=rinv[:, tl:tl + 1], scalar2=None, op0=ALU.mult),
                                 reads=[bankB[ab], rvB], writes=[oaB[T % 2]])

                    def kinds(j, i):
                        if i == j:
                            return Dp[:, h, 0, :]
                        if i == j + 1:
                            return Dp[:, h, 1, :]
                        return None
                    mT = mnegT2[T % 2]
                    attention(T, h, h, qaT[:, h // 2, :], qaB, kaT2, kaB, vaug, vaB, list(range(4 * T + 4)),
                              lambda j: mT[:, j, :], mtB2[T % 2], kinds, writer)

                def idx_steps(i):
                    return ((i + 1 + 3) // 4) * 8 + 1 + NIT + (i + 8) // 8

                BG["width"] = 2
                BG["gens"] = [dsa_indexer(tl) for tl in range(4)]
                bg_drain()
                for T in range(8):
                    if T < 7:
                        BG["gens"] = [dsa_indexer(4 * (T + 1) + tl) for tl in range(4)]
                        nsteps = sum(idx_steps(4 * (T + 1) + tl) for tl in range(4)) + 8
                        BG["rate"] = nsteps / float(8 * (4 * T + 4) - 4)
                    for h in range(8):
                        dsa_head(T, h)
                    bg_drain()
                    K.dma(oa_scr[:, 4 * T:4 * T + 4, :], oa_st[T % 2], reads=[oaB[T % 2]])
                    if DEBUG:
                        K.dma(dbv[:, 4 * T:4 * T + 4, :], oa_st[T % 2], reads=[oaB[T % 2]])
                K.barrier()
                A.release(mB)
                def nsa_phase():
                    wfb = din("wfb", [10, 128, 1024])
                    wtbd = din("wtb", [128, 8 * 280])
                    bcgd = din("bcg", [128, 8 * 503])
                    bcmd = din("bcm", [128, 503])
                    ovd = din("ov", [128, 2 * 64])
                    m0d = din("selm0", [128, 126])
                    m1d = din("selm1", [128, 126])
                    wfard = din("wfar", [128, 128])
                    eexpd = din("eexp", [64, S])
                    w1kd = din("w1k", [128, 32 * 128])
                    w1vd = din("w1v", [128, 32 * 128])
                    w2kd = din("w2k", [128, 128])
                    w2vd = din("w2v", [128, 64])
                    pekd = din("pek", [128, 32])
                    pevd = din("pev", [128, 32])
                    mC = A.mark()
                    Bc = A.alloc([128, 8, 503], BF16)
                    ov = A.alloc([128, 2, 64], BF16)
                    selm0 = A.alloc([128, 126], F32)
                    selm1 = A.alloc([128, 126], F32)
                    wfar = A.alloc([128, 128], BF16)
                    ncB = Buf("nsac")
                    qbT = A.alloc([128, 4, S], BF16)
                    ksT4 = A.alloc([128, 2, S], BF16)
                    kwT4 = A.alloc([128, 2, S], BF16)
                    vaug_s = A.alloc([128, NT, 2, 65], BF16)
                    vaug_w = A.alloc([128, NT, 2, 65], BF16)
                    gates = A.alloc([128, NT, 24], F32)
                    kcmpT = A.alloc([128, 2, 256], BF16)
                    vcmp = A.alloc([128, 2, 2, 64], BF16)
                    qbB, ksB, kwB, vsB, vwB, gtB, kcB, vcB = (Buf(n) for n in ("qbT", "ksT4", "kwT4", "vaug_s", "vaug_w", "gates", "kcmpT", "vcmp"))
                    mC1 = A.mark()
                    t_bcg = A.alloc([128, 8, 503], F32)
                    t_bcm = A.alloc([128, 503], F32)
                    t_ov = A.alloc([128, 128], F32)
                    t_wf = A.alloc([128, 128], F32)
                    K.dma(t_bcg, bcgd.rearrange("p (a b) -> p a b", b=503), writes=[ncB])
                    K.dma(t_bcm, bcmd, writes=[ncB])
                    K.dma(t_ov, ovd, writes=[ncB])
                    K.dma(t_wf, wfard, writes=[ncB])
                    K.dma(selm0, m0d, writes=[ncB])
                    K.dma(selm1, m1d, writes=[ncB])
                    K.op("dve", lambda e: e.tensor_tensor(out=Bc, in0=t_bcg, in1=t_bcm.unsqueeze(1).to_broadcast([128, 8, 503]), op=ALU.add),
                         reads=[ncB], writes=[ncB])
                    K.op("dve", lambda e: e.tensor_copy(out=ov, in_=t_ov.rearrange("p (a b) -> p a b", b=64)), reads=[ncB], writes=[ncB])
                    K.op("dve", lambda e: e.tensor_copy(out=wfar, in_=t_wf), reads=[ncB], writes=[ncB])
                    K.barrier()
                    A.release(mC1)
                    kcT = A.alloc([128, S], BF16)
                    vcT = A.alloc([128, S], BF16)
                    kctB, vctB = Buf("kcT"), Buf("vcT")
                    wtb_ = A.alloc([128, 8, 280], BF16)
                    wst = A.alloc([128, 1024], F32)
                    wb = [A.alloc([128, 1024], BF16)] * 2
                    wstB, wbB = Buf("wst2"), [Buf("wb20")] * 2
                    mC2 = A.mark()
                    wtf = A.alloc([128, 8 * 280], F32)
                    wtB = Buf("wtb")
                    K.dma(wtf, wtbd, writes=[wtB])
                    K.op("pool", lambda e: e.tensor_copy(out=wtb_, in_=wtf.rearrange("p (a b) -> p a b", b=280)), reads=[wtB], writes=[wtB])
                    K.barrier()
                    A.release(mC2)
                    hT = A.alloc([128, 8, S], BF16)
                    hTB = Buf("hT2")
                    mC3 = A.mark()
                    wk2 = {"sq": A.alloc([128, DM], BF16), "ss": A.alloc([128, 1], F32), "rs": A.alloc([128, 1], F32),
                           "xn": A.alloc([128, DM], BF16), "B": Buf("nwk2"), "bank": 0}
                    compute_hT(hT, hTB, wk2, nbuf=1)
                    K.barrier()
                    A.release(mC3)
                    dsts = [(qbT[:, b_, :], qbB, 0.125) for b_ in range(4)] + \
                           [(ksT4[:, 0, :], ksB, 1.0), (ksT4[:, 1, :], ksB, 1.0), (kwT4[:, 0, :], kwB, 1.0), (kwT4[:, 1, :], kwB, 1.0),
                            (kcT, kctB, 1.0), (vcT, vctB, 1.0)]
                    for b_, (dst, dB, sc) in enumerate(dsts):
                        proj_fm(wfb[b_], hT, hTB, dst, dB, wst, wstB, wb[b_ % 2], wbB[b_ % 2], sc, [1, 2])
                    K.op("pool", lambda e: e.memset(vaug_s[:, :, :, 64:65], 1.0), writes=[vsB])
                    K.op("pool", lambda e: e.memset(vaug_w[:, :, :, 64:65], 1.0), writes=[vwB])
                    for i in range(NT):
                        bk = [3, 4][i % 2]
                        for c in range(8):
                            K.op("pe", lambda e, bk=bk, c=c, i=i: e.matmul(banks[bk][:, 0:280], hT[:, c, i * 128:(i + 1) * 128], wtb_[:, c, :],
                                                                           start=(c == 0), stop=(c == 7)),
                                 reads=[hTB, wtB], writes=[bankB[bk]], signal=(c == 7))
                        K.op("act", lambda e, bk=bk, i=i: e.activation(out=vaug_s[:, i, :, 0:64],
                                                                        in_=banks[bk][:, 0:128].rearrange("p (a b) -> p a b", b=64), func=AF.Copy),
                             reads=[bankB[bk]], writes=[vsB])
                        K.op("act", lambda e, bk=bk, i=i: e.activation(out=vaug_w[:, i, :, 0:64],
                                                                        in_=banks[bk][:, 128:256].rearrange("p (a b) -> p a b", b=64), func=AF.Copy),
                             reads=[bankB[bk]], writes=[vwB])
                        K.op("act", lambda e, bk=bk, i=i: e.activation(out=gates[:, i, :], in_=banks[bk][:, 256:280], func=AF.Sigmoid),
                             reads=[bankB[bk]], writes=[gtB])
                    K.barrier()
                    A.release(mC2)
                    w1f = A.alloc([128, 32, 128], F32)
                    w1b = A.alloc([128, 32, 128], BF16)
                    w2f = A.alloc([128, 128], F32)
                    w2b = A.alloc([128, 128], BF16)
                    pef = A.alloc([128, 32], F32)
                    peb = A.alloc([128, 32], BF16)
                    b1 = A.alloc([128, 1], F32)
                    zt_ = A.alloc([128, 256], F32)
                    z2_ = A.alloc([128, 256], F32)
                    gT_ = A.alloc([128, 256], BF16)
                    cwB = Buf("cmpw")
                    czB = Buf("cmpz")
                    for kind in range(2):
                        xcT, xB_ = (kcT, kctB) if kind == 0 else (vcT, vctB)
                        K.dma(w1f, (w1kd if kind == 0 else w1vd).rearrange("p (a b) -> p a b", b=128), writes=[cwB])
                        K.dma(pef, pekd if kind == 0 else pevd, writes=[cwB])
                        if kind == 0:
                            K.dma(w2f, w2kd, writes=[cwB])
                        else:
                            K.dma(w2f[:, 0:64], w2vd, writes=[cwB])
                        K.op("pool", lambda e: e.tensor_copy(out=w1b, in_=w1f), reads=[cwB], writes=[cwB])
                        K.op("pool", lambda e: e.tensor_copy(out=w2b, in_=w2f), reads=[cwB], writes=[cwB])
                        K.op("pool", lambda e: e.tensor_copy(out=peb, in_=pef), reads=[cwB], writes=[cwB])
                        for l in range(32):
                            K.op("pe", lambda e, l=l: e.matmul(banks[5][:, 0:1], w1b[0:64, l, :], peb[0:64, l:l + 1], start=(l == 0), stop=(l == 31)),
                                 reads=[cwB], writes=[bankB[5]], signal=(l == 31))
                        K.op("act", lambda e: e.activation(out=b1, in_=banks[5][:, 0:1], func=AF.Copy), reads=[bankB[5]], writes=[czB])
                        for g in range(2):
                            pb = 64 * g
                            for l in range(32):
                                K.op("pe", lambda e, l=l, pb=pb, xcT=xcT: e.matmul(banks[6][:, 0:255], w1b[pb:pb + 64, l, :],
                                                                                  xcT.rearrange("p (c s) -> p c s", s=16)[pb:pb + 64, (l // 16):(l // 16) + 255, l % 16], start=(l == 0), stop=(l == 31)),
                                     reads=[cwB, xB_], writes=[bankB[6]], signal=(l == 31))
                            zz, z2, gT = zt_[:, 0:255], z2_[:, 0:255], gT_[:, 0:255]
                            K.op("act", lambda e: e.activation(out=zz, in_=banks[6][:, 0:255], func=AF.Identity, bias=b1), reads=[bankB[6], czB], writes=[czB])
                            K.op("dve", lambda e: e.tensor_tensor(out=z2, in0=zz, in1=zz, op=ALU.mult), reads=[czB], writes=[czB])
                            K.op("dve", lambda e: e.tensor_scalar(out=z2, in0=z2, scalar1=0.044715, scalar2=1.0, op0=ALU.mult, op1=ALU.add), reads=[czB], writes=[czB])
                            K.op("dve", lambda e: e.tensor_tensor(out=z2, in0=z2, in1=zz, op=ALU.mult), reads=[czB], writes=[czB])
                            K.op("act", lambda e: e.activation(out=z2, in_=z2, func=AF.Sigmoid, scale=1.5957691216057308), reads=[czB], writes=[czB])
                            K.op("dve", lambda e: e.tensor_tensor(out=gT, in0=z2, in1=zz, op=ALU.mult), reads=[czB], writes=[czB])
                            if kind == 0:
                                K.op("pe", lambda e: e.matmul(banks[7][:, 0:255], w2b, gT, start=True, stop=True), reads=[cwB, czB], writes=[bankB[7]])
                                K.op("act", lambda e, g=g: e.activation(out=kcmpT[:, g, 0:255], in_=banks[7][:, 0:255], func=AF.Copy),
                                     reads=[bankB[7]], writes=[kcB])
                            else:
                                for ch in range(2):
                                    m_ = 128 if ch == 0 else 127
                                    K.op("pe", lambda e, ch=ch, m_=m_: e.matmul(banks[7][0:m_, 0:64], gT_[:, ch * 128:ch * 128 + m_], w2b[:, 0:64],
                                                                                start=True, stop=True), reads=[cwB, czB], writes=[bankB[7]])
                                    K.op("act", lambda e, ch=ch, m_=m_, g=g: e.activation(out=vcmp[0:m_, ch, g, :], in_=banks[7][0:m_, 0:64], func=AF.Copy),
                                         reads=[bankB[7]], writes=[vcB])
                    K.barrier()
                    A.release(mC1)
                    Ebuf = [A.alloc([128, 256], F32) for _ in range(2)]
                    Pbuf = [A.alloc([128, 256], BF16) for _ in range(2)]
                    PTs = [A.alloc([128, 2, 128], BF16) for _ in range(2)]
                    PTB = [Buf("PT0"), Buf("PT1")]
                    EB, PB_ = [Buf("E0"), Buf("E1")], [Buf("P0"), Buf("P1")]
                    cs = [A.alloc([128, 8], F32) for _ in range(2)]
                    csB = [Buf("cs0"), Buf("cs1")]
                    scS = A.alloc([128, 64], F32)
                    scW = A.alloc([128, 64], F32)
                    m8 = A.alloc([128, 16], F32)
                    ssB = Buf("selsc")
                    seln = [A.alloc([128, 4, 2, 64], BF16) for _ in range(2)]
                    slB = [Buf("seln0"), Buf("seln1")]
                    Eexp = A.alloc([128, S], BF16)
                    selT = [A.alloc([128, 512], BF16) for _ in range(2)]
                    stB_ = [Buf("selT0"), Buf("selT1")]
                    mtB = Buf("unused_mask")
                    t_ee = A.alloc([128, 2048], F32)
                    K.op("pool", lambda e: e.memset(Eexp, 0.0), writes=[ncB])
                    K.op("pool", lambda e: e.memset(selT[0], 0.0), writes=[stB_[0]])
                    K.op("pool", lambda e: e.memset(selT[1], 0.0), writes=[stB_[1]])
                    for hf in range(2):
                        K.dma(t_ee[0:64, :], eexpd[:, hf * 2048:(hf + 1) * 2048], writes=[ncB])
                        K.op("pool", lambda e, hf=hf: e.tensor_copy(out=Eexp[0:64, hf * 2048:(hf + 1) * 2048], in_=t_ee[0:64, :]), reads=[ncB], writes=[ncB])
                    K.barrier()
                    ob_st = [A.alloc([128, 4, 512], F32) for _ in range(2)]
                    obB = [Buf("obst0"), Buf("obst1")]
                    ob_bf = A.alloc([128, 4, 512], BF16)
                    obfB = Buf("obbf")
                    coef = A.alloc([128, 4], F32)
                    cfB = Buf("coef")
                    AT["pT"] = [A.alloc([128, 512], BF16) for _ in range(4)]
                    AT["pTB"] = [Buf(f"pTn{q}") for q in range(4)]
                    AT["rinv"] = A.alloc([128, 4], F32)
                    AT["rvB"] = Buf("rinvn")
                    AT["abanks"] = [5, 6]
                    AT["sbanks"] = [3, 4]
                    if DEBUG:
                        dbv2 = dbg_ob.rearrange("(n p) d -> p n d", p=128)

                    def compressed_tile(i, tl, par):
                        for g in range(2):
                            K.op("dve", lambda e: e.memset(banks[0][:, 0:320], 0.0), writes=[bankB[0]])
                            yield
                            for r_ in range(4):
                                h = 4 * g + r_
                                q_ = h % 2
                                pb = 64 * (h % 2)
                                bk = [1, 2][h % 2]
                                csq, Eq, Pq = cs[q_], Ebuf[q_], Pbuf[q_]
                                K.op("pe", lambda e, bk=bk, h=h, pb=pb, g=g: e.matmul(banks[bk][:, 0:255], qbT[pb:pb + 64, h // 2, i * 128:(i + 1) * 128],
                                                                                     kcmpT[pb:pb + 64, g, 0:255], start=True, stop=False),
                                     reads=[qbB, kcB], writes=[bankB[bk]], signal=False)
                                K.op("pe", lambda e, bk=bk, h=h: e.matmul(banks[bk][:, 0:255], ident, Bc[:, h, 248 - 8 * i:248 - 8 * i + 255],
                                                                           start=False, stop=True), reads=[ncB, cB], writes=[bankB[bk]])
                                K.op("dve", lambda e, bk=bk, csq=csq: e.tensor_reduce(out=csq[:, 0:1], in_=banks[bk][:, 0:255], axis=AX.X, op=ALU.max),
                                     reads=[bankB[bk]], writes=[csB[q_]])
                                K.op("dve", lambda e, csq=csq: e.tensor_scalar(out=csq[:, 1:2], in0=csq[:, 0:1], scalar1=-1000.0, scalar2=-1.0,
                                                                               op0=ALU.max, op1=ALU.mult), reads=[csB[q_]], writes=[csB[q_]])
                                K.op("act", lambda e, bk=bk, csq=csq, Eq=Eq: e.activation(out=Eq[:, 0:255], in_=banks[bk][:, 0:255], func=AF.Exp, bias=csq[:, 1:2],
                                                                                         accum_out=csq[:, 2:3]), reads=[bankB[bk], csB[q_]], writes=[EB[q_], csB[q_]])
                                K.op("dve", lambda e, csq=csq: e.tensor_scalar(out=csq[:, 3:4], in0=csq[:, 2:3], scalar1=1e-30, scalar2=None, op0=ALU.max),
                                     reads=[csB[q_]], writes=[csB[q_]])
                                K.op("dve", lambda e, csq=csq: e.reciprocal(out=csq[:, 3:4], in_=csq[:, 3:4]), reads=[csB[q_]], writes=[csB[q_]])
                                K.op("dve", lambda e, csq=csq, Eq=Eq, Pq=Pq: e.tensor_scalar(out=Pq[:, 0:255], in0=Eq[:, 0:255], scalar1=csq[:, 3:4], scalar2=None, op0=ALU.mult),
                                     reads=[EB[q_], csB[q_]], writes=[PB_[q_]])
                                yield
                                yield
                                yield
                                pv = banks[7][:, 0:128].bitcast(BF16)
                                K.op("pe", lambda e, Pq=Pq, pv=pv: e.transpose(pv[:, 0:128], Pq[:, 0:128], ident), reads=[PB_[q_], cB], writes=[bankB[7]], signal=False)
                                K.op("pe", lambda e, Pq=Pq, pv=pv: e.transpose(pv[0:127, 128:256], Pq[:, 128:255], ident), reads=[PB_[q_], cB], writes=[bankB[7]])
                                pt = PTs[q_]
                                ptB = PTB[q_]
                                K.op("act", lambda e, pt=pt, pv=pv: e.activation(out=pt[:, 0, :], in_=pv[:, 0:128], func=AF.Copy), reads=[bankB[7]], writes=[ptB])
                                K.op("act", lambda e, pt=pt, pv=pv: e.activation(out=pt[0:127, 1, :], in_=pv[0:127, 128:256], func=AF.Copy), reads=[bankB[7]], writes=[ptB])
                                yield
                                yield
                                for ch in range(2):
                                    m_ = 128 if ch == 0 else 127
                                    K.op("pe", lambda e, pt=pt, ch=ch, m_=m_, r_=r_, g=g: e.matmul(banks[0][:, r_ * 64:(r_ + 1) * 64], pt[0:m_, ch, :], vcmp[0:m_, ch, g, :],
                                                                                                start=False, stop=False, skip_group_check=True),
                                         reads=[ptB, vcB], writes=[bankB[0]], signal=False)
                                for ch in range(2):
                                    m_ = 128 if ch == 0 else 127
                                    K.op("pe", lambda e, pt=pt, ch=ch, m_=m_: e.matmul(
                                        banks[0][:, 256:320], pt[0:m_, ch, :], ov[0:m_, ch, :], start=False, stop=False, skip_group_check=True),
                                        reads=[ptB, ncB], writes=[bankB[0]], signal=(ch == 1))
                                yield
                            for r_ in range(4):
                                h = 4 * g + r_
                                K.op("dve", lambda e, h=h, r_=r_: e.tensor_scalar(out=ob_st[par][:, tl, h * 64:(h + 1) * 64], in0=banks[0][:, r_ * 64:(r_ + 1) * 64],
                                                                                  scalar1=gates[:, i, h:h + 1], scalar2=None, op0=ALU.mult),
                                     reads=[bankB[0], gtB], writes=[obB[par]])
                            lo_ = 62 - 2 * i
                            K.op("dve", lambda e, lo_=lo_: e.tensor_tensor(out=scS, in0=banks[0][:, 256:320], in1=selm0[:, lo_:lo_ + 64], op=ALU.mult),
                                 reads=[bankB[0], ncB], writes=[ssB])
                            K.op("dve", lambda e, lo_=lo_: e.tensor_tensor(out=scS, in0=scS, in1=selm1[:, lo_:lo_ + 64], op=ALU.add), reads=[ssB, ncB], writes=[ssB])
                            K.op("dve", lambda e: e.memset(scS[:, 0:1], 30000.0), writes=[ssB])
                            K.op("dve", lambda e: e.max(out=m8[:, 0:8], in_=scS), reads=[ssB], writes=[ssB])
                            K.op("dve", lambda e: e.match_replace(out=scW, in_to_replace=m8[:, 0:8], in_values=scS, imm_value=-1e30), reads=[ssB], writes=[ssB])
                            K.op("dve", lambda e: e.max(out=m8[:, 8:16], in_=scW), reads=[ssB], writes=[ssB])
                            K.op("dve", lambda e, g=g: e.tensor_scalar(out=seln[par][:, tl, g, :], in0=scS, scalar1=m8[:, 15:16], scalar2=NEG, op0=ALU.is_lt, op1=ALU.mult),
                                 reads=[ssB], writes=[slB[par]])
                            yield

                    def nsa_writer_factory(T, h, gate_off):
                        par = T % 2

                        def writer(ab):
                            accv = banks[ab][:, 0:260].rearrange("p (a b) -> p a b", b=65)
                            rinv, rvB = AT["rinv"], AT["rvB"]
                            K.op("dve", lambda e: e.reciprocal(out=rinv, in_=accv[:, :, 64]), reads=[bankB[ab]], writes=[rvB])
                            K.op("dve", lambda e: e.tensor_tensor(out=coef, in0=rinv, in1=gates[:, 4 * T:4 * T + 4, gate_off + h], op=ALU.mult),
                                 reads=[rvB, gtB], writes=[cfB])
                            for tl in range(4):
                                K.op("dve", lambda e, tl=tl: e.scalar_tensor_tensor(out=ob_st[par][:, tl, h * 64:(h + 1) * 64], in0=accv[:, tl, 0:64],
                                                                                     scalar=coef[:, tl:tl + 1], in1=ob_st[par][:, tl, h * 64:(h + 1) * 64],
                                                                                     op0=ALU.mult, op1=ALU.add),
                                     reads=[bankB[ab], cfB, obB[par]], writes=[obB[par]])
                        return writer

                    def sel_prep(T, g):
                        par = T % 2
                        pv = banks[7][:, 0:256].bitcast(BF16)
                        for tl in range(4):
                            K.op("pe", lambda e, tl=tl: e.transpose(pv[0:64, tl * 128:(tl + 1) * 128], seln[par][:, tl, g, :], ident),
                                 reads=[slB[par], cB], writes=[bankB[7]], signal=(tl == 3))
                        K.op("act", lambda e: e.activation(out=selT[g][0:64, :], in_=pv[0:64, :], func=AF.Copy),
                             reads=[bankB[7]], writes=[stB_[g]])

                    def sel_head(T, h):
                        g = h // 4

                        def kinds(j, i):
                            if i == j:
                                return Dp[:, 8 + h, 0, :]
                            if i == j + 1:
                                return Dp[:, 8 + h, 1, :]
                            return None
                        attention(T, h, 8 + h, qbT[:, h // 2, :], qbB, ksT4[:, g, :], ksB, vaug_s[:, :, g, :], vsB,
                                  list(range(4 * T + 4)), lambda j: (Eexp[:, j * 128:(j + 1) * 128], selT[g]), stB_[g],
                                  kinds, nsa_writer_factory(T, h, 8))

                    def win_head(T, h):
                        g = h // 4

                        def kinds_w(j, i):
                            if i == j:
                                return Dp[:, 8 + h, 0, :]
                            if i == j + 1:
                                return Dp[:, 8 + h, 1, :]
                            if i == j + 4:
                                return wfar
                            return None
                        attention(T, h, 8 + h, qbT[:, h // 2, :], qbB, kwT4[:, g, :], kwB, vaug_w[:, :, g, :], vwB,
                                  list(range(max(0, 4 * T - 4), 4 * T + 4)), lambda j: None, mtB, kinds_w, nsa_writer_factory(T, h, 16), span=4)

                    BG["width"] = 1
                    BG["gens"] = [compressed_tile(tl, tl, 0) for tl in range(4)]
                    bg_drain()
                    for T in range(8):
                        par = T % 2
                        if T < 7:
                            BG["gens"] = [compressed_tile(4 * (T + 1) + tl, tl, (T + 1) % 2) for tl in range(4)]
                            nsteps = 4 * 2 * (2 + 4 * 7) + 8
                            niter = 8 * (4 * T + 4) + sum(min(8, 4 * T + 4) for _ in range(8))
                            BG["rate"] = nsteps / float(niter - 8)
                        for g in range(2):
                            sel_prep(T, g)
                            for r_ in range(4):
                                sel_head(T, 4 * g + r_)
                        for h in range(8):
                            win_head(T, h)
                        bg_drain()
                        K.op("pool", lambda e, par=par: e.tensor_copy(out=ob_bf, in_=ob_st[par]), reads=[obB[par]], writes=[obfB])
                        K.dma(ob_scr[:, 4 * T:4 * T + 4, :], ob_bf, reads=[obfB])
                        if DEBUG:
                            K.dma(dbv2[:, 4 * T:4 * T + 4, :], ob_st[par], reads=[obB[par]])
                    K.barrier()
                    A.release(mC)

                if stage >= 3:
                    nsa_phase()
                else:
                    zt = A.alloc([128, 4, 512], BF16)
                    zB = Buf("zt")
                    K.op("pool", lambda e: e.memset(zt, 0.0), writes=[zB])
                    for T in range(8):
                        K.dma(ob_scr[:, 4 * T:4 * T + 4, :], zt, reads=[zB])


            if stage in (2, 3):
                attn_phases()

            K.barrier()
            mD = A.mark()
            wg = A.alloc([128, 8, 2048], BF16)
            wba = A.alloc([128, 4, DM], BF16)
            wbb = A.alloc([128, 4, DM], BF16)
            wo = A.alloc([128, 8, DM], BF16)
            wDB = Buf("wD")
            mS2 = A.mark()
            stgD = [A.alloc([128, 2048], F32) for _ in range(2)]
            stgB = [Buf("stgD0"), Buf("stgD1")]
            k = 0
            jobs = []
            wgv = wgd.rearrange("(c p) n -> p c n", p=128)
            for c in range(8):
                jobs.append((wgv[:, c, :], wg[:, c, :], 2048))
            for (dr, sb_, nchk) in ((wbad, wba, 4), (wbbd, wbb, 4), (wod, wo, 8)):
                drv = dr.rearrange("(c p) n -> p c n", p=128)
                for c2 in range(nchk // 2):
                    jobs.append((drv[:, 2 * c2:2 * c2 + 2, :], sb_[:, 2 * c2:2 * c2 + 2, :], -1))
            for (src, dst, kind) in jobs:
                s_ = k % 2
                k += 1
                if kind == 2048:
                    K.dma(stgD[s_], src, writes=[stgB[s_]])
                    K.op("pool", lambda e, s_=s_, dst=dst: e.tensor_copy(out=dst, in_=stgD[s_]), reads=[stgB[s_]], writes=[wDB])
                else:
                    sv = stgD[s_].rearrange("p (a b) -> p a b", b=DM)
                    K.dma(sv, src, writes=[stgB[s_]])
                    K.op("pool", lambda e, sv=sv, dst=dst: e.tensor_copy(out=dst, in_=sv), reads=[stgB[s_]], writes=[wDB])
            K.barrier()
            A.release(mS2)
            xtD = [A.alloc([128, DM], F32) for _ in range(2)]
            xtDB = [Buf("xtD0"), Buf("xtD1")]
            hTt = [A.alloc([128, 8, 128], BF16) for _ in range(2)]
            hTtB = [Buf("hTt0"), Buf("hTt1")]
            wkD = {"sq": A.alloc([128, DM], BF16), "ss": A.alloc([128, 1], F32), "rs": A.alloc([128, 1], F32),
                   "xn": A.alloc([128, DM], BF16), "B": Buf("nwkD"), "bank": 0}
            oin = [A.alloc([128, 2, 512], BF16) for _ in range(2)]
            oinB = [Buf("oin0"), Buf("oin1")]
            oT = [A.alloc([128, 8, 128], BF16) for _ in range(2)]
            oTB = [Buf("oT0"), Buf("oT1")]
            sig = [A.alloc([128, 2048], BF16) for _ in range(2)]
            sigB = [Buf("sig0"), Buf("sig1")]
            ya = A.alloc([128, DM], F32)
            mixb = A.alloc([128, DM], BF16)
            mxB = Buf("mix")
            yaB = Buf("ya")
            mixT = A.alloc([128, 8, 128], BF16)
            mTB = Buf("mixT")
            x2o = [A.alloc([128, DM], F32) for _ in range(2)]
            x2oB = [Buf("x2o0"), Buf("x2o1")]
            if DEBUG:
                dbgf = A.alloc([128, 2048], F32)
                dbgB = Buf("dbgf")
            def d_tile(i):
                    a = i % 2
                    K.dma(xtD[a], xv[:, i, :], writes=[xtDB[a]])
                    normT_tile(xtD[a], xtDB[a], gmix_s, hTt[a], hTtB[a], wkD)
                    pv = banks[1][:, 0:512].bitcast(BF16)
                    K.dma(oin[a][:, 0, :], oa_scr[:, i, :], writes=[oinB[a]])
                    K.dma(oin[a][:, 1, :], ob_scr[:, i, :], writes=[oinB[a]])
                    for c in range(8):
                        src = oin[a][:, c // 4, (c % 4) * 128:(c % 4 + 1) * 128]
                        K.op("pe", lambda e, c=c, src=src: e.transpose(pv[:, c * 128:(c + 1) * 128], src, ident),
                             reads=[oinB[a], cB], writes=[bankB[1]], signal=(c == 7))
                    K.op("act", lambda e: e.activation(out=oT[a], in_=pv.rearrange("p (a b) -> p a b", b=128), func=AF.Copy),
                         reads=[bankB[1]], writes=[oTB[a]])
                    for q in range(4):
                        bk = [2, 3][q % 2]
                        for c in range(8):
                            K.op("pe", lambda e, bk=bk, c=c, q=q: e.matmul(banks[bk][:, :], hTt[a][:, c, :], wg[:, c, q * 512:(q + 1) * 512],
                                                                           start=(c == 0), stop=(c == 7)),
                                 reads=[hTtB[a], wDB], writes=[bankB[bk]], signal=(c == 7))
                        if DEBUG and i == 0 and q == 0:
                            K.op("act", lambda e, bk=bk: e.activation(out=dbgf[:, 0:512], in_=banks[bk][:, :], func=AF.Copy), reads=[bankB[bk]], writes=[dbgB])
                            K.dma(dbg_z, dbgf[:, 0:512], reads=[dbgB])
                            K.op("pool", lambda e: e.tensor_copy(out=dbgf[:, 0:1024], in_=hTt[a].rearrange("p a b -> p (a b)")), reads=[hTtB[a]], writes=[dbgB])
                            K.dma(dbg_hT, dbgf[:, 0:1024], reads=[dbgB])
                            K.op("pool", lambda e: e.tensor_copy(out=dbgf, in_=wg[:, 0, :]), reads=[wDB], writes=[dbgB])
                            K.dma(dbg_wg, dbgf, reads=[dbgB])
                        K.op("act", lambda e, bk=bk, q=q: e.activation(out=sig[a][:, q * 512:(q + 1) * 512], in_=banks[bk][:, :], func=AF.Sigmoid),
                             reads=[bankB[bk]], writes=[sigB[a]])
                    yield
                    for br in range(2):
                        wbr = wba if br == 0 else wbb
                        for nh in range(2):
                            bk = [4, 5][nh]
                            for c in range(4):
                                K.op("pe", lambda e, bk=bk, c=c, br=br, nh=nh, wbr=wbr: e.matmul(
                                    banks[bk][:, :], oT[a][:, br * 4 + c, :], wbr[:, c, nh * 512:(nh + 1) * 512], start=(c == 0), stop=(c == 3)),
                                    reads=[oTB[a], wDB], writes=[bankB[bk]], signal=(c == 3))
                            if br == 0:
                                K.op("dve", lambda e, bk=bk, nh=nh: e.tensor_tensor(out=ya[:, nh * 512:(nh + 1) * 512], in0=banks[bk][:, :],
                                                                                     in1=sig[a][:, nh * 512:(nh + 1) * 512], op=ALU.mult),
                                     reads=[bankB[bk], sigB[a]], writes=[yaB])
                            else:
                                K.op("dve", lambda e, bk=bk, nh=nh: e.tensor_tensor(out=banks[bk][:, :], in0=banks[bk][:, :],
                                                                                     in1=sig[a][:, 1024 + nh * 512:1024 + (nh + 1) * 512], op=ALU.mult),
                                     reads=[bankB[bk], sigB[a]], writes=[bankB[bk]])
                                K.op("dve", lambda e, bk=bk, nh=nh: e.tensor_tensor(out=mixb[:, nh * 512:(nh + 1) * 512], in0=banks[bk][:, :],
                                                                                     in1=ya[:, nh * 512:(nh + 1) * 512], op=ALU.add),
                                     reads=[bankB[bk], yaB], writes=[mxB])
                    pv6 = banks[6][:, 0:512].bitcast(BF16)
                    for c in range(8):
                        K.op("pe", lambda e, c=c: e.transpose(pv6[:, c * 128:(c + 1) * 128], mixb[:, c * 128:(c + 1) * 128], ident),
                             reads=[mxB, cB], writes=[bankB[6]], signal=(c == 7))
                    K.op("act", lambda e: e.activation(out=mixT, in_=pv6.rearrange("p (a b) -> p a b", b=128), func=AF.Copy),
                         reads=[bankB[6]], writes=[mTB])
                    for nh in range(2):
                        bk = [7, 4][nh]
                        for c in range(8):
                            K.op("pe", lambda e, bk=bk, c=c, nh=nh: e.matmul(banks[bk][:, :], mixT[:, c, :], wo[:, c, nh * 512:(nh + 1) * 512],
                                                                             start=(c == 0), stop=(c == 7)),
                                 reads=[mTB, wDB], writes=[bankB[bk]], signal=(c == 7))
                        K.op("dve", lambda e, bk=bk, nh=nh, a=a: e.tensor_tensor(out=x2o[a][:, nh * 512:(nh + 1) * 512], in0=banks[bk][:, :],
                                                                                 in1=xtD[a][:, nh * 512:(nh + 1) * 512], op=ALU.add),
                             reads=[bankB[bk], xtDB[a]], writes=[x2oB[a]])
                    K.dma(x2v_w[:, i, :], x2o[a], reads=[x2oB[a]])
                    if DEBUG:
                        K.dma(dbg_x2.rearrange("(n p) d -> p n d", p=128)[:, i, :], x2o[a], reads=[x2oB[a]])
                        K.dma(dbg_ya.rearrange("(n p) d -> p n d", p=128)[:, i, :], ya, reads=[yaB])
                        K.op("pool", lambda e: e.tensor_copy(out=dbgf[:, 0:DM], in_=mixb), reads=[mxB], writes=[dbgB])
                        K.dma(dbg_mix.rearrange("(n p) d -> p n d", p=128)[:, i, :], dbgf[:, 0:DM], reads=[dbgB])
                        K.op("pool", lambda e: e.tensor_copy(out=dbgf, in_=sig[a]), reads=[sigB[a]], writes=[dbgB])
                        K.dma(dbg_sig.rearrange("(n p) d -> p n d", p=128)[:, i, :], dbgf, reads=[dbgB])

            def fin_gen(gn):
                for _ in gn:
                    pass

            g_prev = None
            for i in range(NT):
                gn = d_tile(i)
                next(gn)
                if g_prev is not None:
                    fin_gen(g_prev)
                g_prev = gn
            fin_gen(g_prev)
            A.release(mP)

        x2src = x if stage == 1 else x2d

        K.barrier()
        mE = A.mark()
        w1 = A.alloc([128, 8, DFF], BF16)
        w2 = A.alloc([128, 32, DM], BF16)
        w1B, w2B = Buf("w1"), Buf("w2")
        mS = A.mark()
        stg = [A.alloc([128, 2048], F32) for _ in range(2)]
        stgB = [Buf("stg0"), Buf("stg1")]
        w1v = w1d.rearrange("(c p) f -> p c f", p=128)
        w2v = w2d.rearrange("(c p) n -> p c n", p=128)
        k = 0
        for c in range(8):
            for hf in range(2):
                s_ = k % 2
                K.dma(stg[s_], w1v[:, c, hf * 2048:(hf + 1) * 2048], writes=[stgB[s_]])
                K.op("pool", lambda e, s_=s_, c=c, hf=hf: e.tensor_copy(out=w1[:, c, hf * 2048:(hf + 1) * 2048], in_=stg[s_]),
                     reads=[stgB[s_]], writes=[w1B])
                k += 1
        for fc2 in range(16):
            s_ = k % 2
            K.dma(stg[s_].rearrange("p (a b) -> p a b", b=DM), w2v[:, fc2 * 2:fc2 * 2 + 2, :], writes=[stgB[s_]])
            K.op("pool", lambda e, s_=s_, fc2=fc2: e.tensor_copy(out=w2[:, fc2 * 2:fc2 * 2 + 2, :],
                                                                    in_=stg[s_].rearrange("p (a b) -> p a b", b=DM)),
                 reads=[stgB[s_]], writes=[w2B])
            k += 1

        K.barrier()
        A.release(mS)
        GT = 256
        NG = S // GT
        TPG = GT // 128
        x2t = [A.alloc([128, TPG, DM], F32) for _ in range(2)]
        x2B = [[Buf(f"x2_{a}_{t}") for t in range(TPG)] for a in range(2)]
        h2T = [A.alloc([128, 8, GT], BF16) for _ in range(2)]
        h2B = [Buf("h2T0"), Buf("h2T1")]
        NUR = 6
        uT = A.alloc([128, NUR, GT], BF16)
        uB = [Buf(f"uT{f}") for f in range(NUR)]
        rtmp = [A.alloc([128, GT], F32) for _ in range(2)]
        rtB = [Buf("rt0"), Buf("rt1")]
        wk = {"sq": A.alloc([128, DM], BF16), "ss": A.alloc([128, 1], F32), "rs": A.alloc([128, 1], F32),
              "xn": A.alloc([128, DM], BF16), "B": Buf("nwk"), "bank": 0}
        x3 = [A.alloc([128, DM], F32) for _ in range(2)]
        x3B = [Buf("x3_0"), Buf("x3_1")]
        ss3 = A.alloc([128, 2], F32)
        rs3 = A.alloc([128, 2], F32)
        sB3 = [Buf("s3_0"), Buf("s3_1")]
        ot = [A.alloc([128, DM], F32) for _ in range(2)]
        otB = [Buf("ot0"), Buf("ot1")]
        x2v = x2src.rearrange("(n p) d -> p n d", p=128)
        outv = out.rearrange("(n p) d -> p n d", p=128)
        ubank = [1, 2]
        ybank = [3, 4, 5, 6]
        yi = 0
        ui = 0
        def e_stage1(g):
            a = g % 2
            for t in range(TPG):
                K.dma(x2t[a][:, t, :], x2v[:, g * TPG + t, :], writes=[x2B[a][t]])
                normT_tile(x2t[a][:, t, :], x2B[a][t], gmlp_s, h2T[a][:, :, t * 128:(t + 1) * 128], h2B[a], wk)

        ust = {"ui": 0}
        ybk = [3, 4, 5, 6]

        def e_u(g, f):
            a = g % 2
            bk = ubank[ust["ui"] % 2]
            r_ = ust["ui"] % 2
            ust["ui"] += 1
            for c in range(8):
                K.op("pe", lambda e, bk=bk, c=c: e.matmul(banks[bk][:, 0:GT], w1[:, c, f * 128:(f + 1) * 128],
                                                          h2T[a][:, c, :], start=(c == 0), stop=(c == 7)),
                     reads=[w1B, h2B[a]], writes=[bankB[bk]], signal=(c == 7))
            K.op("act", lambda e, bk=bk, r_=r_: e.activation(out=rtmp[r_], in_=banks[bk][:, 0:GT], func=AF.Relu),
                 reads=[bankB[bk]], writes=[rtB[r_]])
            K.op("dve", lambda e, r_=r_: e.tensor_tensor(out=uT[:, f % NUR, :], in0=rtmp[r_], in1=rtmp[r_], op=ALU.mult),
                 reads=[rtB[r_]], writes=[uB[f % NUR]])

        def e_y(g, f):
            for t in range(TPG):
                for nh in range(2):
                    bk = ybk[t * 2 + nh]
                    K.op("pe", lambda e, bk=bk, t=t, nh=nh: e.matmul(banks[bk][:, :], uT[:, f % NUR, t * 128:(t + 1) * 128],
                                                                     w2[:, f, nh * 512:(nh + 1) * 512],
                                                                     start=(f == 0), stop=(f == 31)),
                         reads=[uB[f % NUR], w2B], writes=[bankB[bk]], signal=(t == TPG - 1 and nh == 1))

        def e_fin(g):
            a = g % 2
            for t in range(TPG):
                p_ = (g * TPG + t) % 2
                for nh in range(2):
                    bk = ybk[t * 2 + nh]
                    K.op("dve", lambda e, bk=bk, p_=p_, nh=nh, t=t: e.tensor_tensor(
                        out=x3[p_][:, nh * 512:(nh + 1) * 512], in0=banks[bk][:, :],
                        in1=x2t[a][:, t, nh * 512:(nh + 1) * 512], op=ALU.add),
                        reads=[bankB[bk], x2B[a][t]], writes=[x3B[p_]])
                K.op("act", lambda e, p_=p_: e.activation(out=wk["sq"], in_=x3[p_], func=AF.Square, accum_out=ss3[:, p_:p_ + 1]),
                     reads=[x3B[p_]], writes=[sB3[p_], wk["B"]])
                rms_scale(ss3[:, p_:p_ + 1], rs3[:, p_:p_ + 1], 1, [sB3[p_]])
                K.op("dve", lambda e, p_=p_: e.scalar_tensor_tensor(out=ot[p_], in0=x3[p_], scalar=rs3[:, p_:p_ + 1], in1=gfin_s,
                                                                     op0=ALU.mult, op1=ALU.mult),
                     reads=[x3B[p_], sB3[p_], cB], writes=[otB[p_]])
                K.dma(outv[:, g * TPG + t, :], ot[p_], reads=[otB[p_]])

        e_stage1(0)
        for g in range(NG):
            e_u(g, 0)
            e_u(g, 1)
            for f in range(32):
                if f + 2 < 32:
                    e_u(g, f + 2)
                e_y(g, f)
                if f == 16 and g + 1 < NG:
                    e_stage1(g + 1)
            e_fin(g)
        A.release(mE)
        stuck, dsig = K.check_deadlock()
        if stuck:
            info = {}
            for e, (p, n) in stuck.items():
                rec = K.ops[e][p]
                info[e] = (p, n, rec[0], [(d.eng, d.idx, d.sem, d.val) for d in rec[2]][:6])
            raise RuntimeError(f"tracker deadlock: {info} done={dsig}")
        K.replay(stack)
    return nc


STAGE = 3
DEBUG = False
_TEST = {}
NITER = 17


def t5_bucket_np(dist):
    n = np.maximum(dist, 0)
    nf = np.maximum(n, 1).astype(np.float32)
    large = 16 + (np.log(nf / np.float32(16)) / np.float32(math.log(128 / 16)) * np.float32(16)).astype(np.int32)
    large = np.minimum(large, 31)
    return np.where(n < 16, n, large)


def fm_block(w, cols):
    blk = w[:, cols]
    return np.ascontiguousarray(blk.reshape(8, 128, 128).transpose(1, 0, 2).reshape(128, 1024))


def host_consts(w, rb):
    f = np.float32
    r = np.arange
    o = {}
    QA, KA, VA, QI, KI, WI, QB, KVB, GB, GBR = 0, 512, 576, 640, 896, 928, 936, 1448, 2216, 2240
    blocks = [r(QA + 128 * b, QA + 128 * (b + 1)) for b in range(4)]
    blocks.append(np.concatenate([r(KA, KA + 64), r(KA, KA + 64)]))
    blocks += [r(QI + 128 * b, QI + 128 * (b + 1)) for b in range(2)]
    blocks.append(np.concatenate([r(KI, KI + 32)] * 4))
    o["wfa"] = np.stack([fm_block(w, c) for c in blocks])
    ta = w[:, np.concatenate([r(VA, VA + 64), r(WI, WI + 8)])]
    o["wta"] = np.ascontiguousarray(ta.reshape(8, 128, 72).transpose(1, 0, 2).reshape(128, 8 * 72))
    o["wg"] = np.ascontiguousarray(w[:, GBR:GBR + 2048])
    sp = r(128)[:, None]
    tp = r(128)[None, :]
    d0 = rb[t5_bucket_np(tp - sp)]
    d1 = rb[t5_bucket_np(128 + tp - sp)]
    dg = np.stack([d0, d1], axis=2)
    o["dgT"] = np.ascontiguousarray(dg.transpose(0, 3, 2, 1).reshape(128, 16 * 2 * 128))
    o["c31"] = np.ascontiguousarray(rb[31:32, :])
    o["cnegT"] = np.where(sp > tp, f(NEG), f(0)).astype(f)
    o["ctok"] = np.where(tp > sp, f(-1e30), f(0)).astype(f)
    o["pow2"] = (2.0 ** (-np.arange(NITER, dtype=np.float64))).astype(f)[None, :]
    if STAGE >= 3:
        KC, VC, KS, VS, KW, VW = (KVB + 128 * k for k in range(6))
        blocks = [r(QB + 128 * b, QB + 128 * (b + 1)) for b in range(4)]
        for base in (KS, KW):
            for g in range(2):
                blocks.append(np.concatenate([r(base + 64 * g, base + 64 * g + 64)] * 2))
        blocks.append(r(KC, KC + 128))
        blocks.append(r(VC, VC + 128))
        o["wfb"] = np.stack([fm_block(w, c) for c in blocks])
        tb = w[:, np.concatenate([r(VS, VS + 128), r(VW, VW + 128), r(GB, GB + 24)])]
        o["wtb"] = np.ascontiguousarray(tb.reshape(8, 128, 280).transpose(1, 0, 2).reshape(128, 8 * 280))
        cc = r(503)[None, :]
        dist = sp - 16 * (cc - 248) - 31
        bcg = rb[t5_bucket_np(dist)][:, :, 8:16]
        o["bcg"] = np.ascontiguousarray(bcg.transpose(0, 2, 1).reshape(128, 8 * 503))
        o["bcm"] = np.where(dist >= 0, f(0), f(NEG)).astype(f)
        c = r(255)[:, None]
        n = r(64)[None, :]
        ovl = np.clip(np.minimum(16 * c + 32, 64 * n + 64) - np.maximum(16 * c, 64 * n), 0, None).astype(f) / f(32)
        ovp = np.zeros((256, 64), f)
        ovp[:255] = ovl
        o["ov"] = np.ascontiguousarray(ovp.reshape(2, 128, 64).transpose(1, 0, 2).reshape(128, 128))
        rel = r(126)[None, :] - 62
        cl = (r(128) // 64)[:, None]
        forced_cur = rel == cl
        forced_prev = rel == cl - 1
        invalid = rel > cl
        o["selm0"] = np.where(forced_cur | forced_prev | invalid, f(0), f(1)).astype(f)
        o["selm1"] = np.where(invalid, f(-1e30), np.where(forced_cur, f(20000), np.where(forced_prev, f(10000), f(0)))).astype(f)
        o["wfar"] = np.where(sp > tp, f(0), f(NEG)).astype(f)
        o["eexp"] = (r(64)[:, None] == (r(S)[None, :] // 64)).astype(f)
    return o


def cmp_consts(w1k, w2k, pek, w1v, w2v, pev):
    o = {}
    for nm, w1, pe in (("k", w1k, pek), ("v", w1v, pev)):
        w1r = w1.reshape(32, 64, 128).transpose(1, 0, 2)
        o["w1" + nm] = np.ascontiguousarray(np.concatenate([w1r, w1r], axis=0).reshape(128, 32 * 128))
        pt = np.ascontiguousarray(pe.T)
        o["pe" + nm] = np.ascontiguousarray(np.concatenate([pt, pt], axis=0))
    o["w2k"] = np.ascontiguousarray(np.concatenate([w2k, w2k], axis=1))
    o["w2v"] = np.ascontiguousarray(w2v)
    return o


def kernel(x, norm_mix, w_in, cmp_pe_k, cmp_w1_k, cmp_w2_k, cmp_pe_v, cmp_w1_v, cmp_w2_v,
           rel_bias, w_branch_a, w_branch_b, w_out, norm_mlp, w_mlp_in, w_mlp_out, norm_final):
    f = np.float32
    x = np.asarray(x, f)
    B = x.shape[0]
    nc = build_program(STAGE)
    common = {
        "gmix": np.ascontiguousarray(np.asarray(norm_mix, f)[0].reshape(8, 128).T),
        "gmlp": np.ascontiguousarray(np.asarray(norm_mlp, f)[0].reshape(8, 128).T),
        "gfin": np.ascontiguousarray(np.asarray(norm_final, f).reshape(1, DM)),
        "w_mlp_in": np.ascontiguousarray(np.asarray(w_mlp_in, f)[0]),
        "w_mlp_out": np.ascontiguousarray(np.asarray(w_mlp_out, f)[0]),
        "ident": np.eye(128, dtype=f),
    }
    if STAGE >= 2:
        common.update(host_consts(np.asarray(w_in, f)[0], np.asarray(rel_bias, f)))
        common["wba"] = np.ascontiguousarray(np.asarray(w_branch_a, f)[0])
        common["wbb"] = np.ascontiguousarray(np.asarray(w_branch_b, f)[0])
        common["wo"] = np.ascontiguousarray(np.asarray(w_out, f)[0])
    if STAGE >= 3:
        common.update(cmp_consts(*(np.asarray(a, f)[0] for a in (cmp_w1_k, cmp_w2_k, cmp_pe_k, cmp_w1_v, cmp_w2_v, cmp_pe_v))))
    if STAGE == 4:
        common.update(_TEST)
    in_maps = []
    for b in range(B):
        m = dict(common)
        m["x"] = np.ascontiguousarray(x[b])
        in_maps.append(m)
    res = run_bass_kernel_spmd(nc, in_maps, core_ids=list(range(B)))
    global _LAST
    _LAST = res
    return np.stack([np.asarray(r["out"], f) for r in res.results], axis=0)
```
